# Optimizing a Trainium2 kernel written in Bass

```python
import math
import jax
import jax.numpy as jnp
from jax import lax
import numpy as np

D_MODEL = 1024
BATCH = 1
SEQ = 16384
DEPTH = 4

D_MIX = D_MODEL
GROUP_W = D_MIX // 4
HEAD_DIM = 64
CHUNK = 128
EPS = 1e-6

RET_HEADS = GROUP_W // HEAD_DIM
ROPE_BASE = 10000.0

SSM_HEADS = GROUP_W // HEAD_DIM
SSM_GROUPS = 2
SSM_STATE = 128
SSM_CONV = 5

HY_CONV = 3
HY_EMB = 33
HY_BANDS = (HY_EMB - 1) // 2
HY_ORDER = 64
HY_INNER = 2
HY_DECAY_TARGET = 1e-2
HY_FAST_PCT = 0.3
HY_SLOW_PCT = 1.5
HY_SHIFT = 0.05

GDN_HEADS = GROUP_W // HEAD_DIM
GDN_CONV = 5

N_EXPERTS = 16
CAPACITY_FACTOR = 2
EXPERT_FF = 1024

PLE_DIM = 256

RET_COLS = 4 * GROUP_W
SSM_XBC = GROUP_W + 2 * SSM_GROUPS * SSM_STATE
SSM_COLS = GROUP_W + SSM_XBC + 2 * SSM_HEADS
HY_COLS = 3 * GROUP_W
GDN_COLS = 4 * GROUP_W + 4 * GDN_HEADS
N_IN = RET_COLS + SSM_COLS + HY_COLS + GDN_COLS

kernel_name = 'hybrid_parallel_heads_encoder'


def _split(x, sizes):
    offs = np.cumsum(sizes)[:-1].tolist()
    return jnp.split(x, offs, axis=-1)


def _flip(a):
    return a[:, ::-1]


def rmsnorm(x, w):
    xf = x.astype(jnp.float32)
    y = xf * lax.rsqrt(jnp.mean(xf * xf, axis=-1, keepdims=True) + EPS)
    return (y * w.astype(jnp.float32)).astype(x.dtype)


def l2norm(x):
    return x * lax.rsqrt(jnp.sum(x * x, axis=-1, keepdims=True) + EPS)


def dwconv_centred(x, w, bias=None):
    k, c = w.shape
    y = lax.conv_general_dilated(x, w[:, None, :].astype(x.dtype), (1,), [((k - 1) // 2, k // 2)],
                                 dimension_numbers=('NWC', 'WIO', 'NWC'), feature_group_count=c)
    return y if bias is None else y + bias


def rotary(x, pos):
    d = x.shape[-1]
    inv = ROPE_BASE ** (-jnp.arange(0, d, 2, dtype=jnp.float32) / d)
    ang = pos[:, None] * inv[None]
    cos, sin = jnp.cos(ang)[None, :, None, :], jnp.sin(ang)[None, :, None, :]
    x1, x2 = x[..., : d // 2], x[..., d // 2:]
    return jnp.concatenate([x1 * cos - x2 * sin, x1 * sin + x2 * cos], axis=-1)


def retention_mixer(cols, gn_w):
    nb, L, _ = cols.shape
    H, d, T, nc = RET_HEADS, HEAD_DIM, CHUNK, L // CHUNK
    q, k, v, g = _split(cols, [GROUP_W] * 4)
    pos = jnp.arange(L, dtype=jnp.float32)
    q = rotary(q.reshape(nb, L, H, d), pos)
    k = rotary(k.reshape(nb, L, H, d), pos) * d ** -0.5
    v = v.reshape(nb, L, H, d)
    lg = jnp.log(1.0 - 2.0 ** (-5.0 - jnp.arange(H, dtype=jnp.float32)))
    t = jnp.arange(T, dtype=jnp.float32)
    dmask = jnp.exp(jnp.abs(t[:, None] - t[None, :])[None] * lg[:, None, None])
    qc, kc, vc = [a.reshape(nb, nc, T, H, d) for a in (q, k, v)]
    s = jnp.einsum('bcihd,bcjhd->bchij', qc, kc) * dmask
    o = jnp.einsum('bchij,bcjhe->bcihe', s, vc)
    pw_tail = jnp.exp((T - t)[:, None] * lg)
    pw_head = jnp.exp(t[:, None] * lg)
    u_fwd = jnp.einsum('bcjhd,bcjhe->bchde', kc * pw_tail[:, :, None], vc)
    u_bwd = jnp.einsum('bcjhd,bcjhe->bchde', kc * pw_head[:, :, None], vc)
    dec = jnp.exp(T * lg)[:, None, None]

    def step(S, u):
        return dec * S + u, S

    s0 = jnp.zeros((nb, H, d, d), jnp.float32)
    _, prev = lax.scan(step, s0, jnp.moveaxis(u_fwd, 1, 0))
    _, nxt = lax.scan(step, s0, jnp.moveaxis(u_bwd, 1, 0), reverse=True)
    prev, nxt = jnp.moveaxis(prev, 0, 1), jnp.moveaxis(nxt, 0, 1)
    o = (o + jnp.einsum('bcihd,bchde->bcihe', qc * pw_head[:, :, None], prev)
           + jnp.einsum('bcihd,bchde->bcihe', qc * pw_tail[:, :, None], nxt))
    o = o.reshape(nb, L, H, d)
    mu = jnp.mean(o, axis=-1, keepdims=True)
    var = jnp.mean(jnp.square(o - mu), axis=-1, keepdims=True)
    o = ((o - mu) * lax.rsqrt(var + EPS)).reshape(nb, L, GROUP_W) * gn_w
    return jax.nn.silu(g) * o


def ssd_chunked(x, dt, A, Bm, Cm):
    nb, L, H, P = x.shape
    N = Bm.shape[-1]
    T, nc = CHUNK, L // CHUNK
    xc = (x * dt[..., None]).reshape(nb, nc, T, H, P)
    Bc = Bm.reshape(nb, nc, T, H, N)
    Cc = Cm.reshape(nb, nc, T, H, N)
    a_cum = jnp.cumsum((dt * A).reshape(nb, nc, T, H), axis=2)
    tri = jnp.tril(jnp.ones((T, T), bool))[None, None, :, :, None]
    seg = a_cum[:, :, :, None, :] - a_cum[:, :, None, :, :]
    lmask = jnp.exp(jnp.where(tri, seg, -jnp.inf))
    scores = jnp.einsum('bcihn,bcjhn->bcijh', Cc, Bc) * lmask
    y = jnp.einsum('bcijh,bcjhp->bcihp', scores, xc)
    decay_to_end = jnp.exp(a_cum[:, :, -1:, :] - a_cum)
    states = jnp.einsum('bcjhn,bcjhp->bchpn', Bc * decay_to_end[..., None], xc)
    chunk_decay = jnp.exp(a_cum[:, :, -1, :])

    def step(S, inp):
        st, dc = inp
        return dc[:, :, None, None] * S + st, S

    _, prev = lax.scan(step, jnp.zeros((nb, H, P, N), jnp.float32),
                       (jnp.moveaxis(states, 1, 0), jnp.moveaxis(chunk_decay, 1, 0)))
    prev = jnp.moveaxis(prev, 0, 1)
    y = y + jnp.einsum('bcihn,bchpn->bcihp', Cc * jnp.exp(a_cum)[..., None], prev)
    return y.reshape(nb, L, H, P)


def mamba2_mixer(cols, conv_w, conv_b, a_log, dt_bias, d_skip, norm_w):
    nb, L, _ = cols.shape
    z, xbc, dt_raw = _split(cols, [GROUP_W, SSM_XBC, 2 * SSM_HEADS])
    xbc = jax.nn.silu(dwconv_centred(xbc, conv_w, conv_b))
    xs, Bm, Cm = _split(xbc, [GROUP_W, SSM_GROUPS * SSM_STATE, SSM_GROUPS * SSM_STATE])
    rep = SSM_HEADS // SSM_GROUPS
    xs = xs.reshape(nb, L, SSM_HEADS, HEAD_DIM)
    Bm = jnp.repeat(Bm.reshape(nb, L, SSM_GROUPS, SSM_STATE), rep, axis=2)
    Cm = jnp.repeat(Cm.reshape(nb, L, SSM_GROUPS, SSM_STATE), rep, axis=2)
    dt_raw = dt_raw.reshape(nb, L, 2, SSM_HEADS)
    dt_f = jax.nn.softplus(dt_raw[:, :, 0] + dt_bias[0])
    dt_b = jax.nn.softplus(dt_raw[:, :, 1] + dt_bias[1])
    y_f = ssd_chunked(xs, dt_f, -jnp.exp(a_log[0]), Bm, Cm)
    y_b = _flip(ssd_chunked(_flip(xs), _flip(dt_b), -jnp.exp(a_log[1]), _flip(Bm), _flip(Cm)))
    y = (y_f + y_b + xs * d_skip[:, None]).reshape(nb, L, GROUP_W)
    return rmsnorm(y * jax.nn.silu(z), norm_w)


def hyena_filters(L, w_in, b_in, w_mid, b_mid, freq, w_out):
    t = jnp.linspace(0.0, 1.0, L, dtype=jnp.float32)[:, None]
    bands = jnp.linspace(1e-4, HY_BANDS - 1, HY_BANDS, dtype=jnp.float32)
    ang = (2.0 * math.pi / L) * jnp.arange(L, dtype=jnp.float32)[:, None] * bands[None]
    z = jnp.concatenate([t, jnp.cos(ang), -jnp.sin(ang)], axis=-1)
    h = jnp.sin(freq * (z @ w_in + b_in))
    for j in range(HY_INNER):
        h = jnp.sin(freq * (h @ w_mid[j] + b_mid[j]))
    h = h @ w_out
    max_decay = math.log(HY_DECAY_TARGET) / HY_FAST_PCT
    min_decay = math.log(HY_DECAY_TARGET) / HY_SLOW_PCT
    deltas = jnp.tile(jnp.linspace(min_decay, max_decay, GROUP_W, dtype=jnp.float32), 2)
    h = h * (jnp.exp(-t * jnp.abs(deltas)) + HY_SHIFT)
    h_fwd, h_bwd = h[:, :GROUP_W], h[:, GROUP_W:]
    return jnp.concatenate([h_fwd, jnp.zeros((1, GROUP_W), jnp.float32), h_bwd[1:][::-1]], axis=0)


def hyena_mixer(cols, conv_w, conv_b, w_in, b_in, w_mid, b_mid, freq, w_out, bias, norm_w):
    L = cols.shape[1]
    u = dwconv_centred(cols, conv_w, conv_b)
    x0, x1, v = _split(u, [GROUP_W] * 3)
    v = v * x1
    filt = hyena_filters(L, w_in, b_in, w_mid, b_mid, freq, w_out)
    v_f = jnp.fft.rfft(v, n=2 * L, axis=1)
    f_f = jnp.fft.rfft(filt, n=2 * L, axis=0)
    y = jnp.fft.irfft(v_f * f_f[None], n=2 * L, axis=1)[:, :L] + v * bias
    return rmsnorm(y * x0, norm_w)


def gated_delta_chunked(q, k, v, g, beta):
    nb, L, H, dk = q.shape
    dv = v.shape[-1]
    T, nc = CHUNK, L // CHUNK

    def chunks(a):
        return jnp.moveaxis(a.reshape(nb, nc, T, H, -1), 3, 2)

    qc, kc, vc = chunks(q), chunks(k), chunks(v)
    gc = jnp.cumsum(chunks(g[..., None])[..., 0], axis=-1)
    bc = chunks(beta[..., None])
    incl = jnp.tril(jnp.ones((T, T), bool))
    strict = jnp.tril(jnp.ones((T, T), bool), -1)
    decay = jnp.exp(jnp.where(incl, gc[..., :, None] - gc[..., None, :], -jnp.inf))
    a_strict = jnp.where(strict, jnp.einsum('bchid,bchjd->bchij', kc * bc, kc) * decay, 0.0)
    eye = jnp.broadcast_to(jnp.eye(T, dtype=jnp.float32), a_strict.shape)
    tinv = lax.linalg.triangular_solve(a_strict, eye, left_side=True, lower=True, unit_diagonal=True)
    u = tinv @ (vc * bc)
    w = tinv @ (kc * bc * jnp.exp(gc)[..., None])
    qk = jnp.where(incl, jnp.einsum('bchid,bchjd->bchij', qc, kc) * decay, 0.0)
    q_dec = qc * jnp.exp(gc)[..., None]
    g_last = gc[..., -1]
    k_dec = kc * jnp.exp(g_last[..., None] - gc)[..., None]

    def step(S, inp):
        q_i, k_i, u_i, w_i, qk_i, gl_i = inp
        v_new = u_i - jnp.einsum('bhtk,bhkv->bhtv', w_i, S)
        o_i = jnp.einsum('bhtk,bhkv->bhtv', q_i, S) + jnp.einsum('bhts,bhsv->bhtv', qk_i, v_new)
        S = S * jnp.exp(gl_i)[..., None, None] + jnp.einsum('bhtk,bhtv->bhkv', k_i, v_new)
        return S, o_i

    xs = tuple(jnp.moveaxis(a, 1, 0) for a in (q_dec, k_dec, u, w, qk, g_last))
    _, o = lax.scan(step, jnp.zeros((nb, H, dk, dv), jnp.float32), xs)
    o = jnp.moveaxis(jnp.moveaxis(o, 0, 1), 2, 3)
    return o.reshape(nb, L, H, dv)


def gdn_mixer(cols, conv_w, a_log, dt_bias, norm_w):
    nb, L, _ = cols.shape
    qkv, z, a_raw, b_raw = _split(cols, [3 * GROUP_W, GROUP_W, 2 * GDN_HEADS, 2 * GDN_HEADS])
    qkv = jax.nn.silu(dwconv_centred(qkv, conv_w))
    q, k, v = [a.reshape(nb, L, GDN_HEADS, HEAD_DIM) for a in _split(qkv, [GROUP_W] * 3)]
    q = l2norm(q) * HEAD_DIM ** -0.5
    k = l2norm(k)
    a_raw = a_raw.reshape(nb, L, 2, GDN_HEADS)
    b_raw = b_raw.reshape(nb, L, 2, GDN_HEADS)
    g_f = -jnp.exp(a_log[0]) * jax.nn.softplus(a_raw[:, :, 0] + dt_bias[0])
    g_b = -jnp.exp(a_log[1]) * jax.nn.softplus(a_raw[:, :, 1] + dt_bias[1])
    beta_f = jax.nn.sigmoid(b_raw[:, :, 0])
    beta_b = jax.nn.sigmoid(b_raw[:, :, 1])
    o = (gated_delta_chunked(q, k, v, g_f, beta_f)
         + _flip(gated_delta_chunked(_flip(q), _flip(k), _flip(v), _flip(g_b), _flip(beta_b))))
    o = rmsnorm(o, norm_w).reshape(nb, L, GROUP_W)
    return o * jax.nn.silu(z)


def expert_choice_moe(x, router_w, w_gate, w_up, w_down):
    nb, L, D = x.shape
    cap = CAPACITY_FACTOR * L // N_EXPERTS
    aff = jax.nn.softmax(jnp.einsum('bld,de->ble', x, router_w).astype(jnp.float32), axis=-1)
    gate, idx = lax.top_k(jnp.swapaxes(aff, 1, 2), cap)
    xs = jax.vmap(lambda xb, ib: xb[ib])(x, idx)
    h = jax.nn.silu(jnp.einsum('becd,edf->becf', xs, w_gate)) * jnp.einsum('becd,edf->becf', xs, w_up)
    ye = jnp.einsum('becf,efd->becd', h, w_down) * gate[..., None].astype(x.dtype)
    return jax.vmap(lambda ib, yb: jnp.zeros((L, D), yb.dtype).at[ib.reshape(-1)].add(yb.reshape(-1, D)))(idx, ye)


def setup_inputs(seed: int = 0) -> dict:
    key = jax.random.key(seed)
    ks = iter(jax.random.split(key, 40))

    def nrm(shape, scale):
        return jax.random.normal(next(ks), shape, jnp.float32) * scale

    def gain(shape):
        return 1.0 + nrm(shape, 0.02)

    def a_log(shape):
        return jnp.log(jax.random.uniform(next(ks), shape, jnp.float32, 1.0, 16.0))

    def dt_bias(shape):
        dt = jnp.exp(jax.random.uniform(next(ks), shape, jnp.float32, math.log(1e-3), math.log(1e-1)))
        return dt + jnp.log(-jnp.expm1(-dt))

    n = DEPTH
    return {
        'x': nrm((BATCH, SEQ, D_MODEL), 1.0),
        'p': nrm((DEPTH, BATCH, SEQ, PLE_DIM), 1.0),
        'norm_mix_w': gain((n, D_MODEL)),
        'w_in': nrm((n, D_MODEL, N_IN), D_MODEL ** -0.5),
        'ret_gn_w': gain((n, GROUP_W)),
        'ssm_conv_w': nrm((n, SSM_CONV, SSM_XBC), SSM_CONV ** -0.5),
        'ssm_conv_b': nrm((n, SSM_XBC), 0.01),
        'ssm_a_log': a_log((n, 2, SSM_HEADS)),
        'ssm_dt_bias': dt_bias((n, 2, SSM_HEADS)),
        'ssm_d': gain((n, SSM_HEADS)),
        'ssm_norm_w': gain((n, GROUP_W)),
        'hy_conv_w': nrm((n, HY_CONV, HY_COLS), HY_CONV ** -0.5),
        'hy_conv_b': nrm((n, HY_COLS), 0.01),
        'hy_filt_w_in': nrm((n, HY_EMB, HY_ORDER), HY_EMB ** -0.5),
        'hy_filt_b_in': nrm((n, HY_ORDER), 0.01),
        'hy_filt_w_mid': nrm((n, HY_INNER, HY_ORDER, HY_ORDER), HY_ORDER ** -0.5),
        'hy_filt_b_mid': nrm((n, HY_INNER, HY_ORDER), 0.01),
        'hy_filt_freq': gain((n, HY_ORDER)),
        'hy_filt_w_out': nrm((n, HY_ORDER, 2 * GROUP_W), HY_ORDER ** -0.5),
        'hy_bias': nrm((n, GROUP_W), 0.1),
        'hy_norm_w': gain((n, GROUP_W)),
        'gdn_conv_w': nrm((n, GDN_CONV, 3 * GROUP_W), GDN_CONV ** -0.5),
        'gdn_a_log': a_log((n, 2, GDN_HEADS)),
        'gdn_dt_bias': dt_bias((n, 2, GDN_HEADS)),
        'gdn_norm_w': gain((n, HEAD_DIM)),
        'w_out': nrm((n, D_MIX, D_MODEL), D_MIX ** -0.5),
        'norm_ffn_w': gain((n, D_MODEL)),
        'router_w': nrm((n, D_MODEL, N_EXPERTS), D_MODEL ** -0.5),
        'exp_w_gate': nrm((n, N_EXPERTS, D_MODEL, EXPERT_FF), D_MODEL ** -0.5),
        'exp_w_up': nrm((n, N_EXPERTS, D_MODEL, EXPERT_FF), D_MODEL ** -0.5),
        'exp_w_down': nrm((n, N_EXPERTS, EXPERT_FF, D_MODEL), EXPERT_FF ** -0.5),
        'ple_norm_w': gain((n, D_MODEL)),
        'ple_gate_w': nrm((n, D_MODEL, D_MODEL), D_MODEL ** -0.5),
        'ple_proj_w': nrm((n, PLE_DIM, D_MODEL), PLE_DIM ** -0.5),
        'final_norm_w': gain((D_MODEL,)),
    }


def reference(x, p, norm_mix_w, w_in, ret_gn_w, ssm_conv_w, ssm_conv_b, ssm_a_log, ssm_dt_bias,
              ssm_d, ssm_norm_w, hy_conv_w, hy_conv_b, hy_filt_w_in, hy_filt_b_in, hy_filt_w_mid,
              hy_filt_b_mid, hy_filt_freq, hy_filt_w_out, hy_bias, hy_norm_w, gdn_conv_w, gdn_a_log,
              gdn_dt_bias, gdn_norm_w, w_out, norm_ffn_w, router_w, exp_w_gate, exp_w_up, exp_w_down,
              ple_norm_w, ple_gate_w, ple_proj_w, final_norm_w):
    h = x
    for i in range(DEPTH):
        hn = rmsnorm(h, norm_mix_w[i])
        cols = jnp.einsum('bld,dn->bln', hn, w_in[i]).astype(jnp.float32)
        c_ret, c_ssm, c_hy, c_gdn = _split(cols, [RET_COLS, SSM_COLS, HY_COLS, GDN_COLS])
        mixed = jnp.concatenate([
            retention_mixer(c_ret, ret_gn_w[i]),
            mamba2_mixer(c_ssm, ssm_conv_w[i], ssm_conv_b[i], ssm_a_log[i], ssm_dt_bias[i],
                         ssm_d[i], ssm_norm_w[i]),
            hyena_mixer(c_hy, hy_conv_w[i], hy_conv_b[i], hy_filt_w_in[i], hy_filt_b_in[i],
                        hy_filt_w_mid[i], hy_filt_b_mid[i], hy_filt_freq[i], hy_filt_w_out[i],
                        hy_bias[i], hy_norm_w[i]),
            gdn_mixer(c_gdn, gdn_conv_w[i], gdn_a_log[i], gdn_dt_bias[i], gdn_norm_w[i]),
        ], axis=-1).astype(h.dtype)
        h = h + jnp.einsum('bln,nd->bld', mixed, w_out[i])
        h = h + expert_choice_moe(rmsnorm(h, norm_ffn_w[i]), router_w[i], exp_w_gate[i],
                                  exp_w_up[i], exp_w_down[i])
        gate = jax.nn.sigmoid(jnp.einsum('bld,de->ble', rmsnorm(h, ple_norm_w[i]), ple_gate_w[i]))
        h = h + gate * jnp.einsum('blk,kd->bld', p[i], ple_proj_w[i])
    return rmsnorm(h, final_norm_w)
```

```python
import contextlib
import os
import numpy as np
import concourse.bass as bass
import concourse.mybir as mybir
from concourse.bass_utils import run_bass_kernel_spmd

F32 = mybir.dt.float32
BF16 = mybir.dt.bfloat16
AF = mybir.ActivationFunctionType
ALU = mybir.AluOpType
AX = mybir.AxisListType

NCORES = 8
DMA_ENGS = ("sync", "pool", "act")
NDSEM = 20


class Prog:
    def __init__(self, name="k"):
        self.nc = bass.Bass("TRN2", target_bir_lowering=False)
        self.stack = contextlib.ExitStack()
        self.engs = ["sync", "act", "dve", "pool", "pe"]
        self.dcnt = {e: 0 for e in DMA_ENGS}
        self.ops = {e: [] for e in self.engs}
        self.cnt = {e: 0 for e in self.engs}
        self.last_w = {}
        self.readers = {}
        self.waited = {e: {} for e in self.engs}
        self.semh = {}
        self.ntiles = 0
        for e in ("act", "dve", "pool", "pe"):
            self.semh[e] = self.stack.enter_context(self.nc.semaphore("s_" + e))
        for e in DMA_ENGS:
            for i in range(NDSEM):
                self.semh[(e, i)] = self.stack.enter_context(self.nc.semaphore("d_%s_%d" % (e, i)))

    def dram(self, name, shape, dtype=F32, kind="Internal"):
        return self.nc.dram_tensor(name, list(shape), dtype, kind=kind).ap()

    def inp(self, name, shape, dtype=F32):
        return self.dram(name, shape, dtype, "ExternalInput")

    def outp(self, name, shape, dtype=F32):
        return self.dram(name, shape, dtype, "ExternalOutput")

    def sbc(self, shape, dtype=F32, name=None):
        if not hasattr(self, "_cache"):
            self._cache = {}
        if name not in self._cache:
            self._cache[name] = self.sb(shape, dtype, name)
        return self._cache[name]

    def sb(self, shape, dtype=F32, name=None):
        self.ntiles += 1
        return self.stack.enter_context(self.nc.sbuf_tensor("sb_" + (name or "t%d" % self.ntiles), list(shape), dtype))

    def ps(self, shape, dtype=F32, name=None):
        self.ntiles += 1
        return self.stack.enter_context(self.nc.psum_tensor("ps_" + (name or "p%d" % self.ntiles), list(shape), dtype))

    def op(self, eng, fn, reads=(), writes=(), dma=False):
        writes = list(writes) + [k for k in reads if isinstance(k, str) and k.startswith("ps")]
        deps = {}

        def add(tok):
            s, v = tok
            if deps.get(s, 0) < v:
                deps[s] = v

        for k in reads:
            if k in self.last_w:
                add(self.last_w[k])
        for k in writes:
            if k in self.last_w:
                add(self.last_w[k])
            for s, v in self.readers.get(k, {}).items():
                add((s, v))
        if dma:
            j = self.dcnt[eng]
            self.dcnt[eng] += 1
            sem = (eng, j % NDSEM)
            val = 16 * (j // NDSEM + 1)
            if j >= NDSEM:
                add((sem, 16 * (j // NDSEM)))
            inc = 16
        else:
            self.cnt[eng] += 1
            sem = eng
            val = self.cnt[eng]
            inc = 1
        waits = []
        for s, v in deps.items():
            if eng == "pe" and s == "pe":
                continue
            if self.waited[eng].get(s, 0) < v:
                waits.append((s, v))
                self.waited[eng][s] = v
        self.ops[eng].append((fn, waits, sem, inc))
        tok = (sem, val)
        for k in reads:
            r = self.readers.setdefault(k, {})
            if r.get(sem, 0) < val:
                r[sem] = val
        for k in writes:
            self.last_w[k] = tok
            self.readers[k] = {}
        return tok

    def i(self, eng, meth, *args, reads=(), writes=(), **kw):
        return self.op(eng, lambda e: getattr(e, meth)(*args, **kw), reads, writes)

    def dma(self, out, in_, reads=(), writes=(), eng="sync", **kw):
        return self.op(eng, lambda e: e.dma_start(out=out, in_=in_, **kw), reads, writes, dma=True)

    def mm(self, out, lhsT, rhs, start=True, stop=True, reads=(), writes=()):
        if getattr(self, "f32r", False) and lhsT.dtype == F32 and rhs.dtype == F32:
            lhsT = lhsT.bitcast(mybir.dt.float32r)
            rhs = rhs.bitcast(mybir.dt.float32r)
        return self.op("pe", lambda e: e.matmul(out, lhsT, rhs, start=start, stop=stop), reads, writes)

    def tr(self, out, in_, ident, reads=(), writes=()):
        return self.op("pe", lambda e: e.transpose(out, in_, ident), reads, writes)

    def act(self, out, in_, func, reads=(), writes=(), **kw):
        return self.op("act", lambda e: e.activation(out=out, in_=in_, func=func, **kw), reads, writes)

    def finish(self):
        for q in DMA_ENGS:
            n = self.dcnt[q]
            waits = []
            for i in range(min(n, NDSEM)):
                last_j = i + NDSEM * ((n - 1 - i) // NDSEM)
                waits.append(((q, i), 16 * (last_j // NDSEM + 1)))
            self.ops[q].append((None, waits, None, 0))
        nc = self.nc
        prog = self

        def run(name, e):
            for fn, waits, sem, inc in prog.ops[name]:
                for s, v in waits:
                    e.wait_ge(prog.semh[s], v)
                if fn is not None:
                    ins = fn(e)
                    ins.then_inc(prog.semh[sem], inc)

        with nc.Block() as block:
            @block.sync
            def _(e):
                run("sync", e)

            @block.scalar
            def _(e):
                run("act", e)

            @block.vector
            def _(e):
                run("dve", e)

            @block.gpsimd
            def _(e):
                run("pool", e)

            @block.tensor
            def _(e):
                run("pe", e)
        self.stack.close()
        return nc


def run_spmd(prog, in_maps):
    nc = prog.finish()
    if os.environ.get("KTRACE"):
        res = run_bass_kernel_spmd(nc, in_maps, core_ids=list(range(NCORES)), trace=True)
        print("KTRACE exec_time_ns", res.exec_time_ns)
    else:
        res = run_bass_kernel_spmd(nc, in_maps, core_ids=list(range(NCORES)))
    return res.results


D = 1024
L = 16384
TPC = L // NCORES
N_IN = 3864


def rmsnorm_fm(P, hT, w_col, out_tiles, ntok, ones, ps_pool, tag, out_keys, h_keys, eps=1e-6):
    nk = len(hT)
    sq = P.sbc([128, 512], F32, name="rms_sq")
    rstd = P.sbc([128, ntok], F32, name="rms_rstd%d" % ntok)
    tag_ = tag
    tag = "rms"
    for n0 in range(0, ntok, 512):
        nn = min(512, ntok - n0)
        pst, psk = ps_pool()
        for k in range(nk):
            P.op("act", lambda e, k=k, n0=n0, nn=nn: e.activation(out=sq[:, :nn], in_=hT[k][:, n0:n0 + nn], func=AF.Square),
                 reads=[h_keys[k]], writes=[tag + "_sq"])
            P.mm(pst[:, :nn], ones[:, :], sq[:, :nn], start=(k == 0), stop=(k == nk - 1), reads=[tag + "_sq", "ones"], writes=[psk])
        P.op("dve", lambda e, n0=n0, nn=nn, pst=pst: e.tensor_scalar(rstd[:, n0:n0 + nn], pst[:, :nn], 1.0 / (128 * nk), eps, op0=ALU.mult, op1=ALU.add),
             reads=[psk], writes=[tag + "_rstd"])
    P.op("act", lambda e: e.activation(out=rstd[:, :], in_=rstd[:, :], func=AF.Sqrt), reads=[tag + "_rstd"], writes=[tag + "_rstd"])
    P.op("dve", lambda e: e.reciprocal(rstd[:, :], rstd[:, :]), reads=[tag + "_rstd"], writes=[tag + "_rstd"])
    for k in range(nk):
        P.op("dve", lambda e, k=k: e.scalar_tensor_tensor(out=out_tiles[k], in0=hT[k], scalar=w_col[:, k:k + 1], in1=rstd[:, :], op0=ALU.mult, op1=ALU.mult),
             reads=[h_keys[k], tag + "_rstd", "wcol_" + tag_], writes=[out_keys[k]])
    return rstd


class PsPool:
    def __init__(self, P, n, shape=(128, 512), dtype=F32, tag="ps"):
        self.t = [(P.ps(list(shape), dtype, name="%s%d" % (tag, i)), "%s%d" % (tag, i)) for i in range(n)]
        self.i = 0

    def __call__(self):
        r = self.t[self.i % len(self.t)]
        self.i += 1
        return r


def build_inproj(layer_tag="l1"):
    P = Prog()
    hT_d = P.inp("hT", [D, TPC])
    nw_d = P.inp("nw", [128, 8])
    win_d = P.inp("w_in", [D, N_IN])
    out_d = P.outp("colsT", [N_IN, TPC])
    ones = P.sb([128, 128], F32, name="ones")
    P.op("dve", lambda e: e.memset(ones[:, :], 1.0), writes=["ones"])
    nw = P.sb([128, 8], F32, name="nw_sb")
    P.dma(nw[:, :], nw_d[:, :], writes=["wcol_n1"])
    psA = PsPool(P, 2, tag="psA")
    psB = PsPool(P, 4, tag="psB")
    wbf = [P.sb([128, N_IN], BF16, name="wbf%d" % k) for k in range(8)]
    wst = [P.sb([128, N_IN], F32, name="wst%d" % i) for i in range(2)]
    for k in range(8):
        st = wst[k % 2]
        P.dma(st[:, :], win_d[k * 128:(k + 1) * 128, :], writes=["wst%d" % (k % 2)], eng="sync" if k % 2 == 0 else "act")
        P.op("pool", lambda e, k=k, st=st: e.tensor_copy(wbf[k][:, :], st[:, :]), reads=["wst%d" % (k % 2)], writes=["wbf%d" % k])
    NT = 1024
    hT = [P.sb([128, NT], F32, name="hT%d" % k) for k in range(8)]
    hn = [P.sb([128, NT], BF16, name="hn%d" % k) for k in range(8)]
    ost = [P.sb([128, 512], F32, name="ost%d" % i) for i in range(4)]
    oi = 0
    for t0 in range(0, TPC, NT):
        for k in range(8):
            P.dma(hT[k][:, :], hT_d[k * 128:(k + 1) * 128, t0:t0 + NT], writes=["hT%d" % k], eng="sync" if k % 2 == 0 else "act")
        rmsnorm_fm(P, [h[:, :] for h in hT], nw, [h[:, :] for h in hn], NT, ones, psA, "n1",
                   ["hn%d" % k for k in range(8)], ["hT%d" % k for k in range(8)])
        for m0 in range(0, N_IN, 128):
            mm_ = min(128, N_IN - m0)
            for n0 in range(0, NT, 512):
                pst, psk = psB()
                for k in range(8):
                    P.mm(pst[:mm_, :], wbf[k][:, m0:m0 + mm_], hn[k][:, n0:n0 + 512], start=(k == 0), stop=(k == 7),
                         reads=["wbf%d" % k, "hn%d" % k], writes=[psk])
                o = ost[oi % 4]
                ok = "ost%d" % (oi % 4)
                if oi % 2 == 0:
                    P.op("act", lambda e, o=o, pst=pst, mm_=mm_: e.copy(o[:mm_, :], pst[:mm_, :]), reads=[psk], writes=[ok])
                else:
                    P.op("dve", lambda e, o=o, pst=pst, mm_=mm_: e.tensor_copy(o[:mm_, :], pst[:mm_, :]), reads=[psk], writes=[ok])
                P.dma(out_d[m0:m0 + mm_, t0 + n0:t0 + n0 + 512], o[:mm_, :], reads=[ok], writes=["out"], eng="sync")
                oi += 1
    return P


import os
SCN = int(os.environ.get("SCN", "16"))
GRP = int(os.environ.get("GRP", "4"))
RD = 2 * GRP + 2
NEG = -1.0e30


class Rot:
    def __init__(self, P, n, shape, dtype, tag):
        self.t = [(P.sb(list(shape), dtype, name="%s%d" % (tag, i)), "%s%d" % (tag, i)) for i in range(n)]
        self.i = 0

    def __call__(self):
        r = self.t[self.i % len(self.t)]
        self.i += 1
        return r


def softplus_tile(P, out, in_, bias_col, shape, tag, rk, wk):
    t = P.sbc(shape, F32, name=tag + "_t")
    a = P.sbc(shape, F32, name=tag + "_a")
    P.op("dve", lambda e: e.tensor_scalar(t[:, :], in_, bias_col, None, op0=ALU.add), reads=rk, writes=[tag + "_t"])
    P.act(a[:, :], t[:, :], AF.Abs, reads=[tag + "_t"], writes=[tag + "_a"])
    P.act(a[:, :], a[:, :], AF.Exp, reads=[tag + "_a"], writes=[tag + "_a"], scale=-1.0)
    P.act(a[:, :], a[:, :], AF.Ln, reads=[tag + "_a"], writes=[tag + "_a"], bias=1.0)
    P.op("dve", lambda e: e.tensor_scalar(t[:, :], t[:, :], 0.0, None, op0=ALU.max), reads=[tag + "_t"], writes=[tag + "_t"])
    P.op("dve", lambda e: e.tensor_tensor(out, t[:, :], a[:, :], op=ALU.add), reads=[tag + "_t", tag + "_a"], writes=wk)


def conv_fm(P, out, xin, w, b, nch, K, width, tag, rk, wk, func):
    acc = P.sbc([128, width], F32, name=tag + "_acc")
    for k in range(K):
        if k == 0:
            P.op("dve", lambda e: e.tensor_scalar(acc[:nch, :], xin[:, 0:width], w[:, 0:1], None, op0=ALU.mult),
                 reads=rk, writes=[tag + "_acc"])
        else:
            P.op("dve", lambda e, k=k: e.scalar_tensor_tensor(out=acc[:nch, :], in0=xin[:, k:k + width], scalar=w[:, k:k + 1], in1=acc[:nch, :], op0=ALU.mult, op1=ALU.add),
                 reads=list(rk) + [tag + "_acc"], writes=[tag + "_acc"])
    if b is None:
        P.act(out, acc[:nch, :], func, reads=[tag + "_acc"], writes=wk)
    else:
        P.act(out, acc[:nch, :], func, reads=[tag + "_acc"], writes=wk, bias=b)


def build_scan(mode, Lx):
    nch = Lx // 128
    nsc = nch // SCN
    W = SCN * 128
    P = Prog()
    P.f32r = bool(int(os.environ.get("F32R", "0")))
    N = 64 if mode in ("ret", "gdn") else 128
    PV = 64
    tri_d = P.inp("tri", [128, 128])
    mneg_d = P.inp("mneg", [128, 128])
    id_d = P.inp("ident", [128, 128])
    out_d = P.outp("o", [128, nch, PV])
    tri = P.sb([128, 128], F32, name="tri")
    mneg = P.sb([128, 128], F32, name="mneg")
    ident = P.sb([128, 128], F32, name="ident")
    ones = P.sb([128, 128], F32, name="ones")
    P.dma(tri[:, :], tri_d[:, :], writes=["tri"])
    P.dma(mneg[:, :], mneg_d[:, :], writes=["mneg"])
    P.dma(ident[:, :], id_d[:, :], writes=["ident"])
    P.op("dve", lambda e: e.memset(ones[:, :], 1.0), writes=["ones"])
    g = P.sb([128, nch], F32, name="g")
    S = P.sb([128, PV], F32, name="S")
    P.op("dve", lambda e: e.memset(S[:, :], 0.0), writes=["S"])

    if mode == "ret":
        names = ["q", "qsw", "k", "ksw", "v", "cq", "sq", "ck", "sk"]
        din = {n: P.inp(n, [128, nch, 64]) for n in names}
        g_d = P.inp("g", [128, nch])
        P.dma(g[:, :], g_d[:, :], writes=["g"])
        tl = {n: [P.sb([128, SCN, 64], F32, name="%s_%d" % (n, i)) for i in range(2)] for n in names}
    elif mode == "ssd":
        xp_d = P.inp("xpad", [64, Lx + 4])
        bp_d = P.inp("bpad", [128, Lx + 4])
        cp_d = P.inp("cpad", [128, Lx + 4])
        prm = {}
        for n, shp in (("cwx", [64, 5]), ("cbx", [64, 1]), ("cwb", [128, 5]), ("cbb", [128, 1]), ("cwc", [128, 5]), ("cbc", [128, 1]),
                       ("dtb", [128, 1]), ("alog", [128, 1]), ("dsk", [128, 1])):
            d = P.inp(n, shp)
            t = P.sb(shp, F32, name=n + "_sb")
            P.dma(t[:, :], d[:, :], writes=[n])
            prm[n] = t
        dtr_d = P.inp("dtraw", [128, nch])
        dtr = P.sb([128, nch], F32, name="dtr")
        dt = P.sb([128, nch], F32, name="dt")
        P.dma(dtr[:, :], dtr_d[:, :], writes=["dtr"])
        softplus_tile(P, dt[:, :], dtr[:, :], prm["dtb"][:, 0:1], [128, nch], "sp", ["dtr", "dtb"], ["dt"])
        ea = P.sb([128, 1], F32, name="ea")
        P.act(ea[:, :], prm["alog"][:, :], AF.Exp, reads=["alog"], writes=["ea"])
        P.op("dve", lambda e: e.tensor_scalar(g[:, :], dt[:, :], ea[:, 0:1], -1.0, op0=ALU.mult, op1=ALU.mult), reads=["dt", "ea"], writes=["g"])
        xin = [P.sb([64, W + 4], F32, name="xin%d" % i) for i in range(2)]
        bin_ = [P.sb([128, W + 4], F32, name="bin%d" % i) for i in range(2)]
        cin = [P.sb([128, W + 4], F32, name="cin%d" % i) for i in range(2)]
        xc = P.sb([64, W], F32, name="xc")
        bc = P.sb([128, W], F32, name="bc")
        cc = P.sb([128, W], F32, name="cc")

    elif mode == "gdn":
        qp_d = P.inp("qpad", [64, Lx + 4])
        kp_d = P.inp("kpad", [64, Lx + 4])
        vp_d = P.inp("vpad", [64, Lx + 4])
        pmT_d = P.inp("pmT", [128, 128])
        pmT = P.sb([128, 128], F32, name="pmT")
        P.dma(pmT[:, :], pmT_d[:, :], writes=["pmT"])
        prm = {}
        for n, shp in (("cwq", [64, 5]), ("cwk", [64, 5]), ("cwv", [64, 5]), ("dtb", [128, 1]), ("alog", [128, 1])):
            d = P.inp(n, shp)
            t = P.sb(shp, F32, name=n + "_sb")
            P.dma(t[:, :], d[:, :], writes=[n])
            prm[n] = t
        ar_d = P.inp("araw", [128, nch])
        br_d = P.inp("braw", [128, nch])
        ar = P.sb([128, nch], F32, name="ar")
        bet = P.sb([128, nch], F32, name="bet")
        dt = P.sb([128, nch], F32, name="dt")
        P.dma(ar[:, :], ar_d[:, :], writes=["ar"])
        P.dma(bet[:, :], br_d[:, :], writes=["bet"])
        P.act(bet[:, :], bet[:, :], AF.Sigmoid, reads=["bet"], writes=["bet"])
        softplus_tile(P, dt[:, :], ar[:, :], prm["dtb"][:, 0:1], [128, nch], "sp", ["ar", "dtb"], ["dt"])
        ea = P.sb([128, 1], F32, name="ea")
        P.act(ea[:, :], prm["alog"][:, :], AF.Exp, reads=["alog"], writes=["ea"])
        P.op("dve", lambda e: e.tensor_scalar(g[:, :], dt[:, :], ea[:, 0:1], -1.0, op0=ALU.mult, op1=ALU.mult), reads=["dt", "ea"], writes=["g"])
        qin = [P.sb([64, W + 4], F32, name="qin%d" % i) for i in range(2)]
        kin = [P.sb([64, W + 4], F32, name="kin%d" % i) for i in range(2)]
        vin = [P.sb([64, W + 4], F32, name="vin%d" % i) for i in range(2)]
        qc = P.sb([64, W], F32, name="qc")
        kc = P.sb([64, W], F32, name="kc")
        vc = P.sb([64, W], F32, name="vc")
        egc = P.sb([128, SCN], F32, name="egc")
        q_t = Rot(P, RD, [128, 64], F32, "q_t")
        k_t = Rot(P, RD, [128, 64], F32, "k_t")
        v_t = Rot(P, RD, [128, 64], F32, "v_t")
        scr = Rot(P, RD, [128, 64], F32, "scr")
        ssq = Rot(P, RD, [128, 2], F32, "ssq")
        kbg = Rot(P, RD, [128, 64], F32, "kbg")
        vbt = Rot(P, RD, [128, 64], F32, "vbt")
        Et = Rot(P, RD, [128, 128], F32, "Et")
        Pm = Rot(P, 3 * GRP, [128, 128], F32, "Pm")
        Qm = Rot(P, 3 * GRP, [128, 128], F32, "Qm")
        Wm = Rot(P, RD, [128, 128], F32, "Wm")
        u_s = Rot(P, RD, [128, 64], F32, "u_s")
        wT_s = Rot(P, RD, [64, 128], F32, "wT_s")
        vn_s = Rot(P, RD, [128, 64], F32, "vn_s")

    psG = PsPool(P, 2, (128, 512), F32, "psG")
    psO = PsPool(P, 1, (128, 512), F32, "psO")
    psU = PsPool(P, 1, (128, 512), F32, "psU")
    psT = PsPool(P, 4, (128, 512), F32, "psT")
    psS = psT
    Gc = P.sb([128, SCN], F32, name="Gc")
    ngc = P.sb([128, SCN], F32, name="ngc")
    gb = Rot(P, RD, [128, 128], F32, "gb")
    Dm = Rot(P, RD, [128, 128], F32, "Dm")
    AT = Rot(P, RD, [128, 128], F32, "AT")
    EG = Rot(P, RD, [128, 128], F32, "EG")
    qd = Rot(P, RD, [128, 128], F32, "qd")
    qTs = Rot(P, RD, [128, 128], F32, "qTs")
    kTs = Rot(P, RD, [128, 128], F32, "kTs")
    ktm = Rot(P, RD, [128, 128], F32, "ktm")
    vtm = Rot(P, RD, [128, PV], F32, "vtm")
    xtm = Rot(P, RD, [128, PV], F32, "xtm")
    kd = Rot(P, RD, [128, 128], F32, "kd")
    gl = Rot(P, RD, [128, 4], F32, "gl")
    osb = [P.sb([128, SCN, PV], F32, name="osb%d" % i) for i in range(2)]

    for s in range(nsc):
        b = s % 2
        if mode == "ret":
            for i, n in enumerate(names):
                P.dma(tl[n][b][:, :, :], din[n][:, s * SCN:(s + 1) * SCN, :], writes=["%s_%d" % (n, b)], eng=("sync", "act")[i % 2])
            for (x, xs, c_, s_) in (("q", "qsw", "cq", "sq"), ("k", "ksw", "ck", "sk")):
                kx, kxs, kc, ks = ["%s_%d" % (n, b) for n in (x, xs, c_, s_)]
                P.op("dve", lambda e, x=x, c_=c_, b=b: e.tensor_tensor(tl[x][b][:, :, :], tl[x][b][:, :, :], tl[c_][b][:, :, :], op=ALU.mult), reads=[kx, kc], writes=[kx])
                P.op("pool", lambda e, xs=xs, s_=s_, b=b: e.tensor_tensor(tl[xs][b][:, :, :], tl[xs][b][:, :, :], tl[s_][b][:, :, :], op=ALU.mult), reads=[kxs, ks], writes=[kxs])
                P.op("dve", lambda e, x=x, xs=xs, b=b: e.tensor_tensor(tl[x][b][:, :, :], tl[x][b][:, :, :], tl[xs][b][:, :, :], op=ALU.add), reads=[kx, kxs], writes=[kx])
        elif mode == "ssd":
            P.dma(xin[b][:, :], xp_d[:, s * W:s * W + W + 4], writes=["xin%d" % b])
            P.dma(bin_[b][:, :], bp_d[:, s * W:s * W + W + 4], writes=["bin%d" % b], eng="act")
            P.dma(cin[b][:, :], cp_d[:, s * W:s * W + W + 4], writes=["cin%d" % b])
            conv_fm(P, xc[:, :], xin[b], prm["cwx"], prm["cbx"][:, 0:1], 64, 5, W, "cv", ["xin%d" % b, "cwx"], ["xc"], AF.Silu)
            conv_fm(P, bc[:, :], bin_[b], prm["cwb"], prm["cbb"][:, 0:1], 128, 5, W, "cv", ["bin%d" % b, "cwb"], ["bc"], AF.Silu)
            conv_fm(P, cc[:, :], cin[b], prm["cwc"], prm["cbc"][:, 0:1], 128, 5, W, "cv", ["cin%d" % b, "cwc"], ["cc"], AF.Silu)
        elif mode == "gdn":
            P.dma(qin[b][:, :], qp_d[:, s * W:s * W + W + 4], writes=["qin%d" % b])
            P.dma(kin[b][:, :], kp_d[:, s * W:s * W + W + 4], writes=["kin%d" % b], eng="act")
            P.dma(vin[b][:, :], vp_d[:, s * W:s * W + W + 4], writes=["vin%d" % b])
            conv_fm(P, qc[:, :], qin[b], prm["cwq"], None, 64, 5, W, "cv", ["qin%d" % b, "cwq"], ["qc"], AF.Silu)
            conv_fm(P, kc[:, :], kin[b], prm["cwk"], None, 64, 5, W, "cv", ["kin%d" % b, "cwk"], ["kc"], AF.Silu)
            conv_fm(P, vc[:, :], vin[b], prm["cwv"], None, 64, 5, W, "cv", ["vin%d" % b, "cwv"], ["vc"], AF.Silu)
        pt, pk = psT()
        P.mm(pt[:, :SCN], tri[:, :], g[:, s * SCN:(s + 1) * SCN], reads=["tri", "g"], writes=[pk])
        P.op("dve", lambda e, pt=pt: e.tensor_copy(Gc[:, :], pt[:, :SCN]), reads=[pk], writes=["Gc"])
        if mode == "gdn":
            P.act(egc[:, :], Gc[:, :], AF.Exp, reads=["Gc"], writes=["egc"])

        def chunk_gen(s, b, c):
                cg = s * SCN + c
                qT, qTk = qTs()
                kT, kTk = kTs()
                if mode == "ret":
                    pt, pk = psT()
                    P.tr(pt[:64, :128], tl["q"][b][:, c, :], ident[:, :], reads=["q_%d" % b, "ident"], writes=[pk])
                    P.act(qT[:64, :], pt[:64, :128], AF.Copy, reads=[pk], writes=[qTk])
                    yield
                    pt, pk = psT()
                    P.tr(pt[:64, :128], tl["k"][b][:, c, :], ident[:, :], reads=["k_%d" % b, "ident"], writes=[pk])
                    P.act(kT[:64, :], pt[:64, :128], AF.Copy, reads=[pk], writes=[kTk])
                    yield
                    qT_ap, kT_ap = qT[:64, :], kT[:64, :]
                    k_tm_ap = tl["k"][b][:, c, :]
                    v_ap = tl["v"][b][:, c, :]
                    rq, rk_, rktm, rv = [qTk], [kTk], ["k_%d" % b], ["v_%d" % b]
                elif mode == "ssd":
                    qT_ap, kT_ap = cc[:, c * 128:(c + 1) * 128], bc[:, c * 128:(c + 1) * 128]
                    rq, rk_ = ["cc"], ["bc"]
                    kt, ktk = ktm()
                    pt, pk = psT()
                    P.tr(pt[:, :128], bc[:, c * 128:(c + 1) * 128], ident[:, :], reads=["bc", "ident"], writes=[pk])
                    P.act(kt[:, :], pt[:, :128], AF.Copy, reads=[pk], writes=[ktk])
                    yield
                    xt, xtk = xtm()
                    vt, vtk = vtm()
                    pt, pk = psT()
                    P.tr(pt[:, :64], xc[:, c * 128:(c + 1) * 128], ident[:64, :64], reads=["xc", "ident"], writes=[pk])
                    P.act(xt[:, :], pt[:, :64], AF.Copy, reads=[pk], writes=[xtk])
                    yield
                    P.op("dve", lambda e, vt=vt, xt=xt, cg=cg: e.tensor_scalar(vt[:, :], xt[:, :], dt[:, cg:cg + 1], None, op0=ALU.mult), reads=[xtk, "dt"], writes=[vtk])
                    k_tm_ap, v_ap = kt[:, :], vt[:, :]
                    rktm, rv = [ktk], [vtk]
                elif mode == "gdn":
                    tms = []
                    for (src, srck, rot) in ((qc, "qc", q_t), (kc, "kc", k_t), (vc, "vc", v_t)):
                        tt, ttk = rot()
                        pt, pk = psT()
                        P.tr(pt[:, :64], src[:, c * 128:(c + 1) * 128], ident[:64, :64], reads=[srck, "ident"], writes=[pk])
                        P.act(tt[:, :], pt[:, :64], AF.Copy, reads=[pk], writes=[ttk])
                        yield
                        tms.append((tt, ttk))
                    (qt_, qtk), (kt_, ktk_), (vt_, vtk_) = tms
                    sc_, sck = scr()
                    ss_, ssk = ssq()
                    P.act(sc_[:, :], qt_[:, :], AF.Square, reads=[qtk], writes=[sck, ssk], accum_out=ss_[:, 0:1])
                    P.act(sc_[:, :], kt_[:, :], AF.Square, reads=[ktk_], writes=[sck, ssk], accum_out=ss_[:, 1:2])
                    P.op("dve", lambda e, ss_=ss_: e.tensor_scalar(ss_[:, :], ss_[:, :], 1e-6, None, op0=ALU.add), reads=[ssk], writes=[ssk])
                    P.act(ss_[:, :], ss_[:, :], AF.Sqrt, reads=[ssk], writes=[ssk])
                    P.op("dve", lambda e, ss_=ss_: e.reciprocal(ss_[:, :], ss_[:, :]), reads=[ssk], writes=[ssk])
                    P.op("dve", lambda e, qt_=qt_, ss_=ss_: e.tensor_scalar(qt_[:, :], qt_[:, :], ss_[:, 0:1], 0.125, op0=ALU.mult, op1=ALU.mult), reads=[qtk, ssk], writes=[qtk])
                    P.op("dve", lambda e, kt_=kt_, ss_=ss_: e.tensor_scalar(kt_[:, :], kt_[:, :], ss_[:, 1:2], None, op0=ALU.mult), reads=[ktk_, ssk], writes=[ktk_])
                    pt, pk = psT()
                    P.tr(pt[:64, :128], qt_[:, :], ident[:, :], reads=[qtk, "ident"], writes=[pk])
                    P.act(qT[:64, :], pt[:64, :128], AF.Copy, reads=[pk], writes=[qTk])
                    yield
                    pt, pk = psT()
                    P.tr(pt[:64, :128], kt_[:, :], ident[:, :], reads=[ktk_, "ident"], writes=[pk])
                    P.act(kT[:64, :], pt[:64, :128], AF.Copy, reads=[pk], writes=[kTk])
                    yield
                    qT_ap, kT_ap = qT[:64, :], kT[:64, :]
                    k_tm_ap = kt_[:, :]
                    rq, rk_, rktm = [qTk], [kTk], [ktk_]
                    kb_, kbk = kbg()
                    vb_, vbk = vbt()
                    P.op("dve", lambda e, kb_=kb_, kt_=kt_, cg=cg, c=c: e.tensor_scalar(kb_[:, :], kt_[:, :], bet[:, cg:cg + 1], egc[:, c:c + 1], op0=ALU.mult, op1=ALU.mult),
                         reads=[ktk_, "bet", "egc"], writes=[kbk])
                    P.op("pool", lambda e, vb_=vb_, vt_=vt_, cg=cg: e.tensor_scalar(vb_[:, :], vt_[:, :], bet[:, cg:cg + 1], None, op0=ALU.mult), reads=[vtk_, "bet"], writes=[vbk])
                gbt, gbk = gb()
                P.op("pool", lambda e, gbt=gbt, cg=cg: e.tensor_scalar(gbt[:, :], ones[:, :], g[:, cg:cg + 1], None, op0=ALU.mult), reads=["ones", "g"], writes=[gbk])
                pG, pGk = psG()
                P.mm(pG[:, :128], gbt[:, :], tri[:, :], reads=[gbk, "tri"], writes=[pGk])
                dm, dmk = Dm()
                P.op("dve", lambda e, dm=dm, pG=pG, c=c: e.scalar_tensor_tensor(out=dm[:, :], in0=pG[:, :128], scalar=Gc[:, c:c + 1], in1=mneg[:, :], op0=ALU.subtract, op1=ALU.add),
                     reads=[pGk, "Gc", "mneg"], writes=[dmk])
                P.act(dm[:, :], dm[:, :], AF.Exp, reads=[dmk], writes=[dmk])
                eg, egk = EG()
                P.act(eg[:N, :], pG[:N, :128], AF.Exp, reads=[pGk], writes=[egk])
                glt, glk = gl()
                P.op("dve", lambda e, glt=glt, pG=pG: e.tensor_copy(glt[:, 0:1], pG[:, 127:128]), reads=[pGk], writes=[glk])
                P.act(glt[:, 1:2], glt[:, 0:1], AF.Exp, reads=[glk], writes=[glk])
                P.act(glt[:, 2:3], Gc[:, c:c + 1], AF.Exp, reads=[glk, "Gc"], writes=[glk], scale=-1.0, bias=glt[:, 0:1])
                if mode == "gdn":
                    et, etk = Et()
                    P.op("dve", lambda e, et=et, pG=pG, c=c: e.scalar_tensor_tensor(out=et[:, :], in0=pG[:, :128], scalar=Gc[:, c:c + 1], in1=pmT[:, :], op0=ALU.subtract, op1=ALU.add),
                         reads=[pGk, "Gc", "pmT"], writes=[etk])
                    P.act(et[:, :], et[:, :], AF.Exp, reads=[etk], writes=[etk], scale=-1.0)
                    pK, pKk = psT()
                    P.mm(pK[:, :128], kT_ap, kT_ap, reads=rk_, writes=[pKk])
                    Qc, Qk = Qm()
                    P.op("dve", lambda e, Qc=Qc, pK=pK, et=et, cg=cg: e.scalar_tensor_tensor(out=Qc[:, :], in0=pK[:, :128], scalar=bet[:, cg:cg + 1], in1=et[:, :], op0=ALU.mult, op1=ALU.mult),
                         reads=[pKk, "bet", etk], writes=[Qk])
                    yield
                    pM, pMk = psT()
                    P.tr(pM[:, :128], Qc[:, :], ident[:, :], reads=[Qk, "ident"], writes=[pMk])
                    Pc, Pk = Pm()
                    P.act(Pc[:, :], pM[:, :128], AF.Copy, reads=[pMk], writes=[Pk])
                    Wc, Wk = Wm()
                    P.op("dve", lambda e, Wc=Wc, pM=pM: e.tensor_tensor(Wc[:, :], ident[:, :], pM[:, :128], op=ALU.subtract), reads=["ident", pMk], writes=[Wk])
                    for lev in range(1, 7):
                        pQ, pQk = psT()
                        P.mm(pQ[:, :128], Pc[:, :], Qc[:, :], reads=[Pk, Qk], writes=[pQk])
                        if lev < 6:
                            pP, pPk = psT()
                            P.mm(pP[:, :128], Qc[:, :], Pc[:, :], reads=[Pk, Qk], writes=[pPk])
                        Qn, Qnk = Qm()
                        P.act(Qn[:, :], pQ[:, :128], AF.Copy, reads=[pQk], writes=[Qnk])
                        if lev < 6:
                            Pn, Pnk = Pm()
                            P.op("dve", lambda e, Pn=Pn, pP=pP: e.tensor_copy(Pn[:, :], pP[:, :128]), reads=[pPk], writes=[Pnk])
                            yield
                            Pc, Pk = Pn, Pnk
                        Qc, Qk = Qn, Qnk
                        pW, pWk = psT()
                        P.mm(pW[:, :128], Qc[:, :], Wc[:, :], reads=[Qk, Wk], writes=[pWk])
                        P.op("dve", lambda e, Wc=Wc, pW=pW: e.tensor_tensor(Wc[:, :], Wc[:, :], pW[:, :128], op=ALU.add), reads=[Wk, pWk], writes=[Wk])
                        yield
                    pu, puk = psT()
                    P.mm(pu[:, :64], Wc[:, :], vb_[:, :], reads=[Wk, vbk], writes=[puk])
                    us, usk = u_s()
                    P.act(us[:, :], pu[:, :64], AF.Copy, reads=[puk], writes=[usk])
                    yield
                    pw, pwk = psT()
                    P.mm(pw[:64, :128], kb_[:, :], Wc[:, :], reads=[kbk, Wk], writes=[pwk])
                    ws, wsk = wT_s()
                    P.act(ws[:, :], pw[:64, :128], AF.Copy, reads=[pwk], writes=[wsk])
                    yield
                pS, pSk = psS()
                P.mm(pS[:, :128], kT_ap, qT_ap, reads=rk_ + rq, writes=[pSk])
                at, atk = AT()
                P.op("dve", lambda e, at=at, pS=pS, dm=dm: e.tensor_tensor(at[:, :], pS[:, :128], dm[:, :], op=ALU.mult), reads=[pSk, dmk], writes=[atk])
                yield
                qdt, qdk = qd()
                P.op("pool", lambda e, qdt=qdt, qT_ap=qT_ap, eg=eg: e.tensor_tensor(qdt[:N, :], qT_ap, eg[:N, :], op=ALU.mult), reads=rq + [egk], writes=[qdk])
                kdt, kdk = kd()
                P.op("dve", lambda e, kdt=kdt, k_tm_ap=k_tm_ap, glt=glt: e.tensor_scalar(kdt[:, :N], k_tm_ap, glt[:, 2:3], None, op0=ALU.mult), reads=rktm + [glk], writes=[kdk])
                yield "B"
                if mode == "gdn":
                    pv, pvk = psT()
                    P.mm(pv[:, :64], ws[:, :], S[:64, :], reads=[wsk, "S"], writes=[pvk])
                    vn, vnk = vn_s()
                    P.op("dve", lambda e, vn=vn, us=us, pv=pv: e.tensor_tensor(vn[:, :], us[:, :], pv[:, :64], op=ALU.subtract), reads=[usk, pvk], writes=[vnk])
                    yield
                    v_ap = vn[:, :]
                    rv = [vnk]
                pO, pOk = psO()
                P.mm(pO[:, :PV], at[:, :], v_ap, start=True, stop=False, reads=[atk] + rv, writes=[pOk])
                P.mm(pO[:, :PV], qdt[:N, :], S[:N, :], start=False, stop=True, reads=[qdk, "S"], writes=[pOk])
                if mode == "ssd":
                    P.op("dve", lambda e, pO=pO, xt=xt, c=c, b=b: e.scalar_tensor_tensor(out=osb[b][:, c, :], in0=xt[:, :], scalar=prm["dsk"][:, 0:1], in1=pO[:, :PV], op0=ALU.mult, op1=ALU.add),
                         reads=[pOk, xtk, "dsk"], writes=["osb%d" % b])
                    yield
                else:
                    P.act(osb[b][:, c, :], pO[:, :PV], AF.Copy, reads=[pOk], writes=["osb%d" % b])
                    yield
                pU, pUk = psU()
                P.mm(pU[:N, :PV], kdt[:, :N], v_ap, reads=[kdk] + rv, writes=[pUk])
                P.op("dve", lambda e, pU=pU, glt=glt: e.scalar_tensor_tensor(out=S[:N, :], in0=S[:N, :], scalar=glt[:N, 1:2], in1=pU[:N, :PV], op0=ALU.mult, op1=ALU.add),
                     reads=["S", glk, pUk], writes=["S"])
                yield

        gens = [chunk_gen(s, b, c) for c in range(SCN)]
        gi, active, waitb = 0, [], []
        while gi < SCN or active or waitb:
            while len(active) < GRP and len(active) + len(waitb) < 2 * GRP and gi < SCN:
                active.append(gens[gi])
                gi += 1
            for gq in list(active):
                r = next(gq, "DONE")
                if r == "B":
                    active.remove(gq)
                    waitb.append(gq)
                elif r == "DONE":
                    active.remove(gq)
            if waitb:
                r = next(waitb[0], "DONE")
                if r == "DONE":
                    waitb.pop(0)
        P.dma(out_d[:, s * SCN:(s + 1) * SCN, :], osb[b][:, :, :], reads=["osb%d" % b], writes=["out"])
    return P


TWO_PI = 6.283185307179586
MAGIC = 12582912.0


def sinred(P, out, ps_ap, freq, fb, nrow, ncol, tag, rk, wk):
    y = P.sbc([128, 512], F32, name=tag + "_y")
    kf = P.sbc([128, 512], F32, name=tag + "_k")
    yk, kk = tag + "_y", tag + "_k"
    P.i("dve", "tensor_scalar", y[:nrow, :ncol], ps_ap, freq, fb, op0=ALU.mult, op1=ALU.add, reads=rk, writes=[yk])
    P.i("dve", "tensor_scalar", kf[:nrow, :ncol], y[:nrow, :ncol], 1.0 / TWO_PI, MAGIC, op0=ALU.mult, op1=ALU.add, reads=[yk], writes=[kk])
    P.i("dve", "tensor_scalar", kf[:nrow, :ncol], kf[:nrow, :ncol], -MAGIC, None, op0=ALU.add, reads=[kk], writes=[kk])
    P.i("dve", "scalar_tensor_tensor", out=y[:nrow, :ncol], in0=kf[:nrow, :ncol], scalar=-TWO_PI, in1=y[:nrow, :ncol], op0=ALU.mult, op1=ALU.add, reads=[yk, kk], writes=[yk])
    P.i("dve", "tensor_scalar", y[:nrow, :ncol], y[:nrow, :ncol], 3.1415925, -3.1415925, op0=ALU.min, op1=ALU.max, reads=[yk], writes=[yk])
    P.act(out, y[:nrow, :ncol], AF.Sin, reads=[yk], writes=wk)


def sinred_gen(P, out, ps_ap, freq, fb, nrow, ncol, tag, rk, wk):
    y = P.sbc([128, 512], F32, name=tag + "_y")
    kf = P.sbc([128, 512], F32, name=tag + "_k")
    yk, kk = tag + "_y", tag + "_k"
    P.i("dve", "tensor_scalar", y[:nrow, :ncol], ps_ap, freq, fb, op0=ALU.mult, op1=ALU.add, reads=rk, writes=[yk])
    yield
    P.i("dve", "tensor_scalar", kf[:nrow, :ncol], y[:nrow, :ncol], 1.0 / TWO_PI, MAGIC, op0=ALU.mult, op1=ALU.add, reads=[yk], writes=[kk])
    P.i("dve", "tensor_scalar", kf[:nrow, :ncol], kf[:nrow, :ncol], -MAGIC, None, op0=ALU.add, reads=[kk], writes=[kk])
    yield
    P.i("dve", "scalar_tensor_tensor", out=y[:nrow, :ncol], in0=kf[:nrow, :ncol], scalar=-TWO_PI, in1=y[:nrow, :ncol], op0=ALU.mult, op1=ALU.add, reads=[yk, kk], writes=[yk])
    P.i("dve", "tensor_scalar", y[:nrow, :ncol], y[:nrow, :ncol], 3.1415925, -3.1415925, op0=ALU.min, op1=ALU.max, reads=[yk], writes=[yk])
    yield
    P.act(out, y[:nrow, :ncol], AF.Sin, reads=[yk], writes=wk)
    yield


def build_hyena(Lx):
    nb = Lx // 128
    P = Prog()
    nc = P.nc
    CH = 32
    ins = {}
    for n, shp in (("x0pad", [CH, Lx + 2]), ("x1pad", [CH, Lx + 2]), ("vpad", [CH, Lx + 2]), ("zT", [33, 2 * Lx]), ("win", [CH, 2 * Lx]),
                   ("ident", [128, 128])):
        ins[n] = P.inp(n, shp)
    prm = {}
    for n, shp in (("cw0", [CH, 3]), ("cw1", [CH, 3]), ("cwv", [CH, 3]), ("cb0", [CH, 1]), ("cb1", [CH, 1]), ("cbv", [CH, 1]),
                   ("w_in", [33, 64]), ("b_in", [64, 1]), ("w_mid0", [64, 64]), ("w_mid1", [64, 64]), ("b_mid0", [64, 1]), ("b_mid1", [64, 1]),
                   ("freq", [64, 1]), ("w_out_f", [64, CH]), ("w_out_b", [64, CH])):
        d = P.inp(n, shp)
        t = P.sb(shp, F32, name=n + "_sb")
        P.dma(t[:, :], d[:, :], writes=[n])
        prm[n] = t
    yr_d = P.outp("yr", [128, CH, nb])
    vv_d = P.outp("vv", [CH, Lx])
    x0_d = P.outp("x0c", [CH, Lx])
    fr_d = P.dram("fr_scratch", [CH, 2 * Lx], BF16)
    ident = P.sb([128, 128], F32, name="ident")
    P.dma(ident[:, :], ins["ident"][:, :], writes=["ident"])
    fb = P.sb([64, 3], F32, name="fb")
    for i, bn in enumerate(("b_in", "b_mid0", "b_mid1")):
        P.i("dve", "tensor_tensor", fb[:, i:i + 1], prm[bn][:, :], prm["freq"][:, :], op=ALU.mult, reads=[bn, "freq"], writes=["fb"])
    psM = PsPool(P, 4, (128, 512), F32, "psM")
    psY = PsPool(P, 2, (128, 512), F32, "psY")
    psT = PsPool(P, 2, (128, 512), F32, "psT")
    NQ = 2
    NQ = 2
    Vt = P.sb([128, CH, nb], BF16, name="Vt")
    SW = min(2048, Lx)
    xin = {n: Rot(P, 1, [CH, SW + 2], F32, n + "in") for n in ("x0pad", "x1pad", "vpad")}
    x0c = Rot(P, 1, [CH, SW], F32, "x0c")
    x1c = P.sb([CH, SW], F32, name="x1c")
    vc = Rot(P, 1, [CH, SW], F32, "vc")
    for s0 in range(0, Lx, SW):
        tl = {}
        for i, n in enumerate(("x0pad", "x1pad", "vpad")):
            t, tk = xin[n]()
            P.dma(t[:, :], ins[n][:, s0:s0 + SW + 2], writes=[tk], eng=("sync", "act")[i % 2])
            tl[n] = (t, tk)
        a0, a0k = x0c()
        v_, vk = vc()
        conv_fm(P, a0[:, :], tl["x0pad"][0], prm["cw0"], prm["cb0"][:, 0:1], CH, 3, SW, "hc", [tl["x0pad"][1], "cw0"], [a0k], AF.Identity)
        conv_fm(P, x1c[:, :], tl["x1pad"][0], prm["cw1"], prm["cb1"][:, 0:1], CH, 3, SW, "hc", [tl["x1pad"][1], "cw1"], ["x1c"], AF.Identity)
        conv_fm(P, v_[:, :], tl["vpad"][0], prm["cwv"], prm["cbv"][:, 0:1], CH, 3, SW, "hc", [tl["vpad"][1], "cwv"], [vk], AF.Identity)
        P.i("dve", "tensor_tensor", v_[:, :], v_[:, :], x1c[:, :], op=ALU.mult, reads=[vk, "x1c"], writes=[vk])
        P.dma(vv_d[:, s0:s0 + SW], v_[:, :], reads=[vk], writes=["vv_d"])
        P.dma(x0_d[:, s0:s0 + SW], a0[:, :], reads=[a0k], writes=["x0_d"], eng="act")
        for c in range(SW // 128):
            pt, pk = psT()
            P.tr(pt[:, :CH], v_[:, c * 128:(c + 1) * 128], ident[:CH, :CH], reads=[vk, "ident"], writes=[pk])
            P.act(Vt[:, :, s0 // 128 + c], pt[:, :CH], AF.Copy, reads=[pk], writes=["Vt"])
    NFG = 3
    zt = Rot(P, NFG + 1, [33, 512], F32, "zt")
    wn = Rot(P, NFG + 1, [CH, 512], F32, "wn")
    hs = [Rot(P, NFG + 1, [64, 512], F32, "h%d" % i) for i in range(3)]
    fo = Rot(P, NFG + 1, [CH, 512], BF16, "fo")
    fq = prm["freq"][:, 0:1]

    def fgen(x0, slot):
        tag = "sr%d" % slot
        z, zk = zt()
        w_, wk_ = wn()
        P.dma(z[:, :], ins["zT"][:, x0:x0 + 512], writes=[zk])
        P.dma(w_[:, :], ins["win"][:, x0:x0 + 512], writes=[wk_], eng="act")
        hprev, hprevk = z, zk
        for li, (wname, kdim) in enumerate((("w_in", 33), ("w_mid0", 64), ("w_mid1", 64))):
            p, pk = psM()
            P.mm(p[:64, :], prm[wname][:, :], hprev[:, :], reads=[wname, hprevk], writes=[pk])
            hcur, hcurk = hs[li]()
            for _ in sinred_gen(P, hcur[:, :], p[:64, :], fq, fb[:, li:li + 1], 64, 512, tag, [pk, "freq", "fb"], [hcurk]):
                yield
            hprev, hprevk = hcur, hcurk
        p, pk = psM()
        wsel = "w_out_f" if x0 < Lx else "w_out_b"
        P.mm(p[:CH, :], prm[wsel][:, :], hprev[:, :], reads=[wsel, hprevk], writes=[pk])
        f_, fk = fo()
        P.i("dve", "tensor_tensor", f_[:, :], p[:CH, :], w_[:, :], op=ALU.mult, reads=[pk, wk_], writes=[fk])
        yield
        P.dma(fr_d[:, x0:x0 + 512], f_[:, :], reads=[fk], writes=["fr_q%d" % (x0 // (2 * Lx // NQ))])

    x0s = list(range(0, 2 * Lx, 512))
    xi = 0
    active = []
    free_slots = list(range(NFG))
    while xi < len(x0s) or active:
        while free_slots and xi < len(x0s):
            sl = free_slots.pop(0)
            active.append((fgen(x0s[xi], sl), sl))
            xi += 1
        for item in list(active):
            gq, sl = item
            if next(gq, "DONE") == "DONE":
                active.remove(item)
                free_slots.append(sl)
    QW = 2 * Lx // NQ
    FS = Rot(P, 2, [128, QW], BF16, "FS")
    yst = P.sb([128, CH, nb], F32, name="yst")
    q_first = (Lx - 128) // QW
    qorder = [q_first] + [q for q in range(NQ) if q != q_first]
    NSPLIT = 4
    for ch in range(CH):
        pY, pYk = psY()
        first = True
        for q in qorder:
            fs, fsk = FS()
            cw = QW // NSPLIT
            for sp in range(NSPLIT):
                c0 = sp * cw
                ncol = cw
                if q == NQ - 1:
                    ncol = min(ncol, QW - 127 - c0)
                src = bass.AP(fr_d.tensor, ch * 2 * Lx + q * QW + c0, [[1, 128], [1, ncol]])
                rk = ["fr_q%d" % q] + (["fr_q%d" % (q + 1)] if (sp == NSPLIT - 1 and q < NQ - 1) else [])
                P.dma(fs[:, c0:c0 + ncol], src, reads=rk, writes=[fsk], eng=("sync", "act", "pool")[sp % 3])
            ds = []
            for off in range(0, QW, 128):
                base2 = q * QW + off
                d = (Lx - base2) // 128 - 1
                if -nb < d < nb:
                    ds.append((d, off))
            if q == q_first:
                ds.sort(key=lambda t: (t[0] != 0, t[0]))
            for d, off in ds:
                a_lo, a_hi = max(0, d), min(nb, nb + d)
                P.mm(pY[:, a_lo:a_hi], fs[:, off:off + 128], Vt[:, ch, a_lo - d:a_hi - d], start=first, stop=False,
                     reads=[fsk, "Vt"], writes=[pYk])
                first = False
        P.act(yst[:, ch, :], pY[:, :nb], AF.Copy, reads=[pYk], writes=["yst"])
    P.dma(yr_d[:, :, :], yst[:, :, :], reads=["yst"], writes=["yr_d"])
    return P


def hyena_consts(Lx):
    f32 = np.float32
    t = np.linspace(0.0, 1.0, Lx, dtype=f32)[:, None]
    bands = np.linspace(1e-4, 15.0, 16, dtype=f32)
    ang = (f32(2.0 * np.pi / Lx) * np.arange(Lx, dtype=f32)[:, None] * bands[None]).astype(f32)
    z = np.concatenate([t, np.cos(ang), -np.sin(ang)], axis=-1).astype(f32)
    max_decay = np.log(1e-2) / 0.3
    min_decay = np.log(1e-2) / 1.5
    deltas = np.abs(np.linspace(min_decay, max_decay, 256, dtype=f32))
    win = (np.exp(-t * deltas[None]) + f32(0.05)).astype(f32)
    m = np.concatenate([Lx - 1 - np.arange(Lx), np.minimum(np.arange(Lx) + 1, Lx - 1)])
    zT = np.ascontiguousarray(z[m].T)
    winx = win[m].copy()
    winx[2 * Lx - 1] = 0.0
    return zT, np.ascontiguousarray(winx.T)


def hyena_inmaps(cT, conv_w, conv_b, w_in, b_in, w_mid, b_mid, freq, w_out, Lx):
    zT, winx = hyena_consts(Lx)
    ident = np.eye(128, dtype=np.float32)
    maps = []
    col = lambda a: np.ascontiguousarray(a.reshape(-1, 1))
    for c in range(NCORES):
        ch = np.arange(32 * c, 32 * c + 32)
        pad = lambda rows: np.ascontiguousarray(np.pad(cT[rows], ((0, 0), (1, 1))))
        m = {"x0pad": pad(ch), "x1pad": pad(256 + ch), "vpad": pad(512 + ch), "zT": zT, "win": np.ascontiguousarray(winx[ch]), "ident": ident,
             "cw0": np.ascontiguousarray(conv_w[:, ch].T), "cw1": np.ascontiguousarray(conv_w[:, 256 + ch].T), "cwv": np.ascontiguousarray(conv_w[:, 512 + ch].T),
             "cb0": col(conv_b[ch]), "cb1": col(conv_b[256 + ch]), "cbv": col(conv_b[512 + ch]),
             "w_in": np.ascontiguousarray(w_in), "b_in": col(b_in), "w_mid0": np.ascontiguousarray(w_mid[0]), "w_mid1": np.ascontiguousarray(w_mid[1]),
             "b_mid0": col(b_mid[0]), "b_mid1": col(b_mid[1]), "freq": col(freq),
             "w_out_f": np.ascontiguousarray(w_out[:, ch]), "w_out_b": np.ascontiguousarray(w_out[:, 256 + ch])}
        maps.append(m)
    return maps


def hyena_collect(res, Lx):
    nb = Lx // 128
    ys, vs, xs = [], [], []
    for c in range(NCORES):
        yr = res[c]["yr"]
        y = yr[::-1].transpose(2, 0, 1).reshape(Lx, 32)
        ys.append(y)
        vs.append(res[c]["vv"].T)
        xs.append(res[c]["x0c"].T)
    return np.concatenate(ys, 1), np.concatenate(vs, 1), np.concatenate(xs, 1)


def bc3(ap2, n):
    return ap2.unsqueeze(2).to_broadcast([ap2.shape[0], ap2.shape[1], n])


def rstd_small(P, t, key, scale, eps=1e-6):
    P.i("dve", "tensor_scalar", t, t, scale, eps, op0=ALU.mult, op1=ALU.add, reads=[key], writes=[key])
    P.act(t, t, AF.Sqrt, reads=[key], writes=[key])
    P.i("dve", "reciprocal", t, t, reads=[key], writes=[key])


def build_lt():
    P = Prog()
    NTL = TPC // 128
    hT_d = P.inp("hT", [D, TPC])
    names = ["ret_f", "ret_b", "ssd_f", "ssd_b", "hy_y", "hy_vv", "hy_x0", "gdn_f", "gdn_b", "g_ret", "z_ssd", "z_gdn"]
    din = {n: P.inp(n, [128, NTL, 256]) for n in names}
    rows = {}
    for n in ("gn_w", "ssm_nw", "hy_nw", "hy_bias", "gdn_nw"):
        d = P.inp(n, [128, 256])
        t = P.sb([128, 256], F32, name=n + "_sb")
        P.dma(t[:, :], d[:, :], writes=[n])
        rows[n] = t
    wout_d = P.inp("w_out", [D, D])
    nw_d = P.inp("nw", [128, 8])
    rw_d = P.inp("rw", [128, 8, 16])
    id_d = P.inp("ident", [128, 128])
    h1_d = P.outp("h1T", [D, TPC])
    aff_d = P.outp("aff", [128, NTL, 16])
    ident = P.sb([128, 128], F32, name="ident")
    P.dma(ident[:, :], id_d[:, :], writes=["ident"])
    ones = P.sb([128, 128], F32, name="ones")
    P.i("dve", "memset", ones[:, :], 1.0, writes=["ones"])
    nw = P.sb([128, 8], F32, name="nw_sb")
    P.dma(nw[:, :], nw_d[:, :], writes=["wcol_n2"])
    rw = P.sb([128, 8, 16], F32, name="rw_sb")
    P.dma(rw[:, :, :], rw_d[:, :, :], writes=["rw"])
    wbf = [P.sb([128, D], BF16, name="wobf%d" % k) for k in range(8)]
    wst = [P.sb([128, D], F32, name="wost%d" % i) for i in range(2)]
    for k in range(8):
        P.dma(wst[k % 2][:, :], wout_d[k * 128:(k + 1) * 128, :], writes=["wost%d" % (k % 2)], eng=("sync", "act")[k % 2])
        P.i("pool", "tensor_copy", wbf[k][:, :], wst[k % 2][:, :], reads=["wost%d" % (k % 2)], writes=["wobf%d" % k])
    psA = PsPool(P, 2, tag="psA")
    psB = PsPool(P, 3, tag="psB")
    psT = PsPool(P, 3, tag="psT")
    NT = 1024
    hT = [P.sb([128, NT], F32, name="hT%d" % k) for k in range(8)]
    xn = [P.sb([128, NT], F32, name="xn%d" % k) for k in range(8)]
    mT = [P.sb([128, NT], BF16, name="mT%d" % k) for k in range(8)]
    inb = {n: Rot(P, 2, [128, 256], F32, "i_" + n) for n in names}
    mixed = Rot(P, 2, [128, 1024], F32, "mixed")
    tmp = Rot(P, 2, [128, 256], F32, "tmp")
    sg = Rot(P, 2, [128, 256], F32, "sg")
    st4 = Rot(P, 4, [128, 4], F32, "st4")
    st1 = Rot(P, 4, [128, 1], F32, "st1")
    affs = P.sb([128, NTL, 16], F32, name="affs")
    for t0 in range(0, TPC, NT):
        for k in range(8):
            P.dma(hT[k][:, :], hT_d[k * 128:(k + 1) * 128, t0:t0 + NT], writes=["hT%d" % k], eng=("sync", "act")[k % 2])
        for tt in range(NT // 128):
            tg = t0 // 128 + tt
            L_ = {}
            for i, n in enumerate(names):
                t, tk = inb[n]()
                P.dma(t[:, :], din[n][:, tg, :], writes=[tk], eng=("sync", "act", "pool")[i % 3])
                L_[n] = (t, tk)
            mx, mxk = mixed()
            a, ak = L_["ret_f"]
            b_, bk = L_["ret_b"]
            P.i("dve", "tensor_tensor", a[:, :], a[:, :], b_[:, :], op=ALU.add, reads=[ak, bk], writes=[ak])
            a3 = a[:, :].rearrange("p (h d) -> p h d", h=4)
            s4, s4k = st4()
            P.i("dve", "tensor_reduce", s4[:, :], a3, axis=AX.X, op=ALU.add, reads=[ak], writes=[s4k])
            P.i("dve", "tensor_scalar", s4[:, :], s4[:, :], 1.0 / 64, None, op0=ALU.mult, reads=[s4k], writes=[s4k])
            P.i("dve", "tensor_tensor", a3, a3, bc3(s4[:, :], 64), op=ALU.subtract, reads=[ak, s4k], writes=[ak])
            tp, tpk = tmp()
            P.i("pool", "tensor_tensor", tp[:, :], a[:, :], a[:, :], op=ALU.mult, reads=[ak], writes=[tpk])
            v4, v4k = st4()
            P.i("dve", "tensor_reduce", v4[:, :], tp[:, :].rearrange("p (h d) -> p h d", h=4), axis=AX.X, op=ALU.add, reads=[tpk], writes=[v4k])
            rstd_small(P, v4[:, :], v4k, 1.0 / 64)
            P.i("dve", "tensor_tensor", a3, a3, bc3(v4[:, :], 64), op=ALU.mult, reads=[ak, v4k], writes=[ak])
            g_, gk = L_["g_ret"]
            s_, sk = sg()
            P.act(s_[:, :], g_[:, :], AF.Silu, reads=[gk], writes=[sk])
            P.i("pool", "tensor_tensor", a[:, :], a[:, :], rows["gn_w"][:, :], op=ALU.mult, reads=[ak, "gn_w"], writes=[ak])
            P.i("dve", "tensor_tensor", mx[:, 0:256], a[:, :], s_[:, :], op=ALU.mult, reads=[ak, sk], writes=[mxk])
            a, ak = L_["ssd_f"]
            b_, bk = L_["ssd_b"]
            z_, zk = L_["z_ssd"]
            P.i("dve", "tensor_tensor", a[:, :], a[:, :], b_[:, :], op=ALU.add, reads=[ak, bk], writes=[ak])
            s_, sk = sg()
            P.act(s_[:, :], z_[:, :], AF.Silu, reads=[zk], writes=[sk])
            P.i("dve", "tensor_tensor", a[:, :], a[:, :], s_[:, :], op=ALU.mult, reads=[ak, sk], writes=[ak])
            tp, tpk = tmp()
            s1, s1k = st1()
            P.act(tp[:, :], a[:, :], AF.Square, reads=[ak], writes=[tpk, s1k], accum_out=s1[:, 0:1])
            rstd_small(P, s1[:, :], s1k, 1.0 / 256)
            P.i("dve", "scalar_tensor_tensor", out=mx[:, 256:512], in0=a[:, :], scalar=s1[:, 0:1], in1=rows["ssm_nw"][:, :], op0=ALU.mult, op1=ALU.mult,
                reads=[ak, s1k, "ssm_nw"], writes=[mxk])
            a, ak = L_["hy_y"]
            v_, vk = L_["hy_vv"]
            x_, xk = L_["hy_x0"]
            P.i("pool", "tensor_tensor", v_[:, :], v_[:, :], rows["hy_bias"][:, :], op=ALU.mult, reads=[vk, "hy_bias"], writes=[vk])
            P.i("dve", "tensor_tensor", a[:, :], a[:, :], v_[:, :], op=ALU.add, reads=[ak, vk], writes=[ak])
            P.i("dve", "tensor_tensor", a[:, :], a[:, :], x_[:, :], op=ALU.mult, reads=[ak, xk], writes=[ak])
            tp, tpk = tmp()
            s1, s1k = st1()
            P.act(tp[:, :], a[:, :], AF.Square, reads=[ak], writes=[tpk, s1k], accum_out=s1[:, 0:1])
            rstd_small(P, s1[:, :], s1k, 1.0 / 256)
            P.i("dve", "scalar_tensor_tensor", out=mx[:, 512:768], in0=a[:, :], scalar=s1[:, 0:1], in1=rows["hy_nw"][:, :], op0=ALU.mult, op1=ALU.mult,
                reads=[ak, s1k, "hy_nw"], writes=[mxk])
            a, ak = L_["gdn_f"]
            b_, bk = L_["gdn_b"]
            z_, zk = L_["z_gdn"]
            P.i("dve", "tensor_tensor", a[:, :], a[:, :], b_[:, :], op=ALU.add, reads=[ak, bk], writes=[ak])
            a3 = a[:, :].rearrange("p (h d) -> p h d", h=4)
            tp, tpk = tmp()
            P.i("pool", "tensor_tensor", tp[:, :], a[:, :], a[:, :], op=ALU.mult, reads=[ak], writes=[tpk])
            v4, v4k = st4()
            P.i("dve", "tensor_reduce", v4[:, :], tp[:, :].rearrange("p (h d) -> p h d", h=4), axis=AX.X, op=ALU.add, reads=[tpk], writes=[v4k])
            rstd_small(P, v4[:, :], v4k, 1.0 / 64)
            P.i("dve", "tensor_tensor", a3, a3, bc3(v4[:, :], 64), op=ALU.mult, reads=[ak, v4k], writes=[ak])
            s_, sk = sg()
            P.act(s_[:, :], z_[:, :], AF.Silu, reads=[zk], writes=[sk])
            P.i("pool", "tensor_tensor", a[:, :], a[:, :], rows["gdn_nw"][:, :], op=ALU.mult, reads=[ak, "gdn_nw"], writes=[ak])
            P.i("dve", "tensor_tensor", mx[:, 768:1024], a[:, :], s_[:, :], op=ALU.mult, reads=[ak, sk], writes=[mxk])
            for k in range(8):
                pt, pk = psT()
                P.tr(pt[:, :128], mx[:, k * 128:(k + 1) * 128], ident[:, :], reads=[mxk, "ident"], writes=[pk])
                if k % 2 == 0:
                    P.act(mT[k][:, tt * 128:(tt + 1) * 128], pt[:, :128], AF.Copy, reads=[pk], writes=["mT%d" % k])
                else:
                    P.i("dve", "tensor_copy", mT[k][:, tt * 128:(tt + 1) * 128], pt[:, :128], reads=[pk], writes=["mT%d" % k])
        for m in range(8):
            for n0 in range(0, NT, 512):
                p, pk = psB()
                for k in range(8):
                    P.mm(p[:, :], wbf[k][:, m * 128:(m + 1) * 128], mT[k][:, n0:n0 + 512], start=(k == 0), stop=(k == 7),
                         reads=["wobf%d" % k, "mT%d" % k], writes=[pk])
                P.i("dve", "tensor_tensor", hT[m][:, n0:n0 + 512], hT[m][:, n0:n0 + 512], p[:, :], op=ALU.add, reads=["hT%d" % m, pk], writes=["hT%d" % m])
            P.dma(h1_d[m * 128:(m + 1) * 128, t0:t0 + NT], hT[m][:, :], reads=["hT%d" % m], writes=["h1_d"], eng=("sync", "act")[m % 2])
        rmsnorm_fm(P, [h[:, :] for h in hT], nw, [x[:, :] for x in xn], NT, ones, psA, "n2",
                   ["xn%d" % k for k in range(8)], ["hT%d" % k for k in range(8)])
        for tt in range(NT // 128):
            tg = t0 // 128 + tt
            p, pk = psT()
            for k in range(8):
                P.mm(p[:, :16], xn[k][:, tt * 128:(tt + 1) * 128], rw[:, k, :], start=(k == 0), stop=(k == 7), reads=["xn%d" % k, "rw"], writes=[pk])
            m1, m1k = st1()
            P.i("dve", "tensor_reduce", m1[:, :], p[:, :16], axis=AX.X, op=ALU.max, negate=True, reads=[pk], writes=[m1k])
            s1, s1k = st1()
            P.act(affs[:, tg, :], p[:, :16], AF.Exp, reads=[pk, m1k], writes=["affs", s1k], bias=m1[:, 0:1], accum_out=s1[:, 0:1])
            P.i("dve", "reciprocal", s1[:, :], s1[:, :], reads=[s1k], writes=[s1k])
            P.i("dve", "tensor_scalar", affs[:, tg, :], affs[:, tg, :], s1[:, 0:1], None, op0=ALU.mult, reads=["affs", s1k], writes=["affs"])
    P.dma(aff_d[:, :, :], affs[:, :, :], reads=["affs"], writes=["aff_d"])
    return P


def tm_tiles(a, ntile):
    return np.ascontiguousarray(a.reshape(ntile, 128, -1).transpose(1, 0, 2))


def un_tm(a):
    return a.transpose(1, 0, 2).reshape(a.shape[0] * a.shape[1], -1)


def bcast_rows(v, n=128):
    return np.ascontiguousarray(np.broadcast_to(v.reshape(1, -1), (n, v.size)))


def lt_inmaps(hT, arr, gn_w, ssm_nw, hy_nw, hy_bias, gdn_nw, w_out, nfw, rw):
    maps = []
    ident = np.eye(128, dtype=np.float32)
    NTL = TPC // 128
    for c in range(NCORES):
        sl = slice(c * TPC, (c + 1) * TPC)
        m = {"hT": np.ascontiguousarray(hT[:, sl]), "w_out": w_out, "ident": ident,
             "nw": np.ascontiguousarray(nfw.reshape(8, 128).T), "rw": np.ascontiguousarray(rw.reshape(8, 128, 16).transpose(1, 0, 2)),
             "gn_w": bcast_rows(gn_w), "ssm_nw": bcast_rows(ssm_nw), "hy_nw": bcast_rows(hy_nw), "hy_bias": bcast_rows(hy_bias),
             "gdn_nw": bcast_rows(np.tile(gdn_nw, 4))}
        for n, a in arr.items():
            m[n] = tm_tiles(a[sl], NTL)
        maps.append(m)
    return maps


def lt_collect(res):
    h1T = np.concatenate([r["h1T"] for r in res], axis=1)
    aff = np.concatenate([un_tm(r["aff"]) for r in res], axis=0)
    return h1T, aff


CAP = 2 * L // 16
NBIS = 28


def build_le(final):
    P = Prog()
    h1_d = P.inp("h1T", [D, TPC])
    affall_d = P.inp("aff_all", [128, 16, L // 128])
    affT_d = P.inp("affT", [16, TPC])
    id_d = P.inp("ident", [128, 128])
    nw_d = P.inp("nw", [128, 8])
    pnw_d = P.inp("pnw", [128, 8])
    wg_d = P.inp("wg", [16, D, D])
    wu_d = P.inp("wu", [16, D, D])
    wd_d = P.inp("wd", [16, D, D])
    pg_d = P.inp("pgw", [D, D])
    pp_d = P.inp("ppw", [256, D])
    pT_d = P.inp("pT", [256, TPC])
    out_d = P.outp("h3T", [D, TPC])
    if final:
        fnw_d = P.inp("fnw", [128, 8])
    ident = P.sb([128, 128], F32, name="ident")
    P.dma(ident[:, :], id_d[:, :], writes=["ident"])
    ones = P.sb([128, 128], F32, name="ones")
    P.i("dve", "memset", ones[:, :], 1.0, writes=["ones"])
    nw = P.sb([128, 8], F32, name="nw_sb")
    P.dma(nw[:, :], nw_d[:, :], writes=["wcol_n2"])
    pnw = P.sb([128, 8], F32, name="pnw_sb")
    P.dma(pnw[:, :], pnw_d[:, :], writes=["wcol_n3"])
    if final:
        fnw = P.sb([128, 8], F32, name="fnw_sb")
        P.dma(fnw[:, :], fnw_d[:, :], writes=["wcol_n4"])
    selt = Rot(P, 2, [16, 128], F32, "selt")
    psA = PsPool(P, 2, tag="psA")
    psB = PsPool(P, 4, tag="psB")
    psC = PsPool(P, 2, tag="psC")
    NB = L // 128
    big = P.sb([128, 2, 16 * NB], F32, name="big")
    aall = big[:, 0, :].rearrange("p (e n) -> p e n", e=16)
    cmp_ = big[:, 1, :].rearrange("p (e n) -> p e n", e=16)
    P.dma(aall, affall_d[:, :, :], writes=["aall"])
    lo = P.sb([128, 16], F32, name="lo")
    hi = P.sb([128, 16], F32, name="hi")
    mid = P.sb([128, 16], F32, name="mid")
    cnt = P.sb([128, 16], F32, name="cnt")
    ge = P.sb([128, 16], F32, name="ge")
    d1 = P.sb([128, 16], F32, name="d1")
    P.i("dve", "memset", lo[:, :], 0.0, writes=["lo"])
    P.i("dve", "memset", hi[:, :], 2.0, writes=["hi"])
    P.i("dve", "memset", mid[:, :], 0.5, writes=["mid"])
    for it in range(NBIS):
        P.i("dve", "tensor_tensor", cmp_, aall, bc3(mid[:, :], NB), op=ALU.is_ge, reads=["aall", "mid"], writes=["cmp"])
        P.i("dve", "tensor_reduce", cnt[:, :], cmp_, axis=AX.X, op=ALU.add, reads=["cmp"], writes=["cnt"])
        p, pk = psC()
        P.mm(p[:, :16], ones[:, :], cnt[:, :], reads=["ones", "cnt"], writes=[pk])
        P.i("dve", "tensor_scalar", ge[:, :], p[:, :16], CAP - 0.5, None, op0=ALU.is_ge, reads=[pk], writes=["ge"])
        P.i("dve", "tensor_tensor", d1[:, :], mid[:, :], lo[:, :], op=ALU.subtract, reads=["mid", "lo"], writes=["d1"])
        P.i("dve", "tensor_tensor", d1[:, :], d1[:, :], ge[:, :], op=ALU.mult, reads=["d1", "ge"], writes=["d1"])
        P.i("dve", "tensor_tensor", lo[:, :], lo[:, :], d1[:, :], op=ALU.add, reads=["lo", "d1"], writes=["lo"])
        P.i("dve", "tensor_tensor", d1[:, :], hi[:, :], mid[:, :], op=ALU.subtract, reads=["mid", "hi"], writes=["d1"])
        P.i("dve", "tensor_tensor", d1[:, :], d1[:, :], ge[:, :], op=ALU.mult, reads=["d1", "ge"], writes=["d1"])
        P.i("dve", "tensor_tensor", hi[:, :], mid[:, :], d1[:, :], op=ALU.add, reads=["mid", "d1"], writes=["hi"])
        P.i("dve", "tensor_tensor", mid[:, :], lo[:, :], hi[:, :], op=ALU.add, reads=["lo", "hi"], writes=["mid"])
        P.i("dve", "tensor_scalar", mid[:, :], mid[:, :], 0.5, None, op0=ALU.mult, reads=["mid"], writes=["mid"])
    thc = P.sb([16, 1], F32, name="thc")
    P.i("dve", "tensor_tensor", d1[:16, :], lo[:16, :], ident[:16, :16], op=ALU.mult, reads=["lo", "ident"], writes=["d1"])
    P.i("dve", "tensor_reduce", thc[:, :], d1[:16, :], axis=AX.X, op=ALU.add, reads=["d1"], writes=["thc"])
    gwT = P.sb([16, TPC], F32, name="gwT")
    P.dma(gwT[:, :], affT_d[:, :], writes=["gwT"])
    P.i("dve", "scalar_tensor_tensor", out=gwT[:, :], in0=gwT[:, :], scalar=thc[:, 0:1], in1=gwT[:, :], op0=ALU.is_ge, op1=ALU.mult, reads=["gwT", "thc"], writes=["gwT"])
    NT = 1024
    hT = [P.sb([128, NT], F32, name="hT%d" % k) for k in range(8)]
    xn = [P.sb([128, NT], BF16, name="xn%d" % k) for k in range(8)]
    wbuf = Rot(P, 4, [128, 8, D], BF16, "wbuf")
    wstg = Rot(P, 6, [128, D], F32, "wstg")
    actb = Rot(P, 2, [128, 8, 512], BF16, "actb")
    gwb = Rot(P, 2, [128, 512], F32, "gwb")
    sgl = Rot(P, 2, [128, 512], F32, "sgl")
    u2 = Rot(P, 2, [128, 512], F32, "u2")
    cvi = [0]

    def load_w(src, e):
        wb, wbk = wbuf()
        for k in range(8):
            st, stk = wstg()
            P.dma(st[:, :], src[e, k * 128:(k + 1) * 128, :], writes=[stk], eng="sync")
            ce = ("dve", "act", "pool", "dve", "act")[cvi[0] % 5]
            cvi[0] += 1
            if ce == "act":
                P.act(wb[:, k, :], st[:, :], AF.Copy, reads=[stk], writes=[wbk])
            else:
                P.i(ce, "tensor_copy", wb[:, k, :], st[:, :], reads=[stk], writes=[wbk])
        return wb, wbk

    for t0 in range(0, TPC, NT):
        for k in range(8):
            P.dma(hT[k][:, :], h1_d[k * 128:(k + 1) * 128, t0:t0 + NT], writes=["hT%d" % k], eng=("sync", "act")[k % 2])
        rmsnorm_fm(P, [h[:, :] for h in hT], nw, [x[:, :] for x in xn], NT, ones, psA, "n2",
                   ["xn%d" % k for k in range(8)], ["hT%d" % k for k in range(8)])
        for e in range(16):
            wg, wgk = load_w(wg_d, e)
            wu, wuk = load_w(wu_d, e)
            wd, wdk = load_w(wd_d, e)
            se, sek = selt()
            P.i("dve", "tensor_scalar", se[:, :], ones[:16, :], ident[:16, e:e + 1], None, op0=ALU.mult, reads=["ones", "ident"], writes=[sek])
            for n0 in range(0, NT, 512):
                pb, pbk = psC()
                P.mm(pb[:, :], se[:, :], gwT[:, t0 + n0:t0 + n0 + 512], reads=[sek, "gwT"], writes=[pbk])
                gb, gbk = gwb()
                P.act(gb[:, :], pb[:, :], AF.Copy, reads=[pbk], writes=[gbk])
                ab, abk = actb()
                for f in range(8):
                    pg, pgk = psB()
                    for k in range(8):
                        P.mm(pg[:, :], wg[:, k, f * 128:(f + 1) * 128], xn[k][:, n0:n0 + 512], start=(k == 0), stop=(k == 7), reads=[wgk, "xn%d" % k], writes=[pgk])
                    pu, puk = psB()
                    for k in range(8):
                        P.mm(pu[:, :], wu[:, k, f * 128:(f + 1) * 128], xn[k][:, n0:n0 + 512], start=(k == 0), stop=(k == 7), reads=[wuk, "xn%d" % k], writes=[puk])
                    s_, sk = sgl()
                    P.act(s_[:, :], pg[:, :], AF.Silu, reads=[pgk], writes=[sk])
                    u_, uk = u2()
                    P.i("dve", "tensor_tensor", u_[:, :], pu[:, :], gb[:, :], op=ALU.mult, reads=[puk, gbk], writes=[uk])
                    P.i("dve", "tensor_tensor", ab[:, f, :], s_[:, :], u_[:, :], op=ALU.mult, reads=[sk, uk], writes=[abk])
                for m in range(8):
                    pd, pdk = psB()
                    for f in range(8):
                        P.mm(pd[:, :], wd[:, f, m * 128:(m + 1) * 128], ab[:, f, :], start=(f == 0), stop=(f == 7), reads=[wdk, abk], writes=[pdk])
                    P.i("dve", "tensor_tensor", hT[m][:, n0:n0 + 512], hT[m][:, n0:n0 + 512], pd[:, :], op=ALU.add, reads=[pdk, "hT%d" % m], writes=["hT%d" % m])
        rmsnorm_fm(P, [h[:, :] for h in hT], pnw, [x[:, :] for x in xn], NT, ones, psA, "n3",
                   ["xn%d" % k for k in range(8)], ["hT%d" % k for k in range(8)])
        pgw, pgwk = load_w(pg_d.rearrange("(o a) b -> o a b", o=1), 0)
        if t0 == 0:
            ppw = P.sb([128, 2, D], BF16, name="ppw")
            for k in range(2):
                st, stk = wstg()
                P.dma(st[:, :], pp_d[k * 128:(k + 1) * 128, :], writes=[stk])
                P.i("pool", "tensor_copy", ppw[:, k, :], st[:, :], reads=[stk], writes=["ppw"])
            pTb = P.sb([128, 2, NT], BF16, name="pTb")
        pst = big[:, 0, 0:NT]
        for k in range(2):
            P.dma(pst, pT_d[k * 128:(k + 1) * 128, t0:t0 + NT], reads=["cmp"], writes=["aall"])
            P.act(pTb[:, k, :], pst, AF.Copy, reads=["aall"], writes=["pTb"])
        for m in range(8):
            for n0 in range(0, NT, 512):
                pg, pgk = psB()
                for k in range(8):
                    P.mm(pg[:, :], pgw[:, k, m * 128:(m + 1) * 128], xn[k][:, n0:n0 + 512], start=(k == 0), stop=(k == 7), reads=[pgwk, "xn%d" % k], writes=[pgk])
                pp, ppk = psB()
                for k in range(2):
                    P.mm(pp[:, :], ppw[:, k, m * 128:(m + 1) * 128], pTb[:, k, n0:n0 + 512], start=(k == 0), stop=(k == 1), reads=["ppw", "pTb"], writes=[ppk])
                s_, sk = sgl()
                P.act(s_[:, :], pg[:, :], AF.Sigmoid, reads=[pgk], writes=[sk])
                u_, uk = u2()
                P.i("dve", "tensor_tensor", u_[:, :], pp[:, :], s_[:, :], op=ALU.mult, reads=[ppk, sk], writes=[uk])
                P.i("pool", "tensor_tensor", hT[m][:, n0:n0 + 512], hT[m][:, n0:n0 + 512], u_[:, :], op=ALU.add, reads=["hT%d" % m, uk], writes=["hT%d" % m])
        if final:
            rmsnorm_fm(P, [h[:, :] for h in hT], fnw, [h[:, :] for h in hT], NT, ones, psA, "n4",
                       ["hT%d" % k for k in range(8)], ["hT%d" % k for k in range(8)])
            for m in range(8):
                P.dma(out_d[m * 128:(m + 1) * 128, t0:t0 + NT], hT[m][:, :], reads=["hT%d" % m], writes=["out_d"], eng=("sync", "act")[m % 2])
        else:
            for m in range(8):
                P.dma(out_d[m * 128:(m + 1) * 128, t0:t0 + NT], hT[m][:, :], reads=["hT%d" % m], writes=["out_d"], eng=("sync", "act")[m % 2])
    return P


def le_inmaps(h1T, aff, nfw, pnw, wg, wu, wd, pgw, ppw, pT, fnw=None):
    ident = np.eye(128, dtype=np.float32)
    aff_all = np.ascontiguousarray(aff.reshape(128, L // 128, 16).transpose(0, 2, 1))
    affT = aff.T
    colw = lambda w: np.ascontiguousarray(w.reshape(8, 128).T)
    maps = []
    for c in range(NCORES):
        sl = slice(c * TPC, (c + 1) * TPC)
        m = {"h1T": np.ascontiguousarray(h1T[:, sl]), "aff_all": aff_all, "affT": np.ascontiguousarray(affT[:, sl]), "ident": ident,
             "nw": colw(nfw), "pnw": colw(pnw), "wg": wg, "wu": wu, "wd": wd, "pgw": pgw, "ppw": ppw, "pT": np.ascontiguousarray(pT[:, sl])}
        if fnw is not None:
            m["fnw"] = colw(fnw)
        maps.append(m)
    return maps


def _colw(w):
    return np.ascontiguousarray(w.reshape(8, 128).T)


def _scan_consts():
    tri = np.triu(np.ones((128, 128), np.float32))
    ident = np.eye(128, dtype=np.float32)
    mneg_incl = np.where(tri > 0, 0.0, NEG).astype(np.float32)
    mneg_strict = np.where(np.triu(np.ones((128, 128), np.float32), 1) > 0, 0.0, NEG).astype(np.float32)
    pmT = np.where(np.tril(np.ones((128, 128), np.float32), -1) > 0, 0.0, -NEG).astype(np.float32)
    return tri, ident, mneg_incl, mneg_strict, pmT


def _rope_tables():
    inv = (10000.0 ** (-np.arange(0, 64, 2, dtype=np.float32) / 64)).astype(np.float32)
    ang = (np.arange(L, dtype=np.float32)[:, None] * inv[None]).astype(np.float32)
    cos, sin = np.cos(ang), np.sin(ang)
    cos2 = np.concatenate([cos, cos], 1).astype(np.float32)
    sinS = np.concatenate([-sin, sin], 1).astype(np.float32)
    return cos2, sinS


def _fm_pad(rows_fm, flip, pad):
    a = rows_fm[:, ::-1] if flip else rows_fm
    return np.ascontiguousarray(np.pad(a, ((0, 0), (pad, pad))))


def _col_tm(v, flip):
    if flip:
        v = v[::-1]
    return np.ascontiguousarray(v.reshape(L // 128, 128).T)


def _full(v):
    return np.full((128, 1), v, np.float32)


def run_ret(colsT):
    tri, ident, mi, ms, _ = _scan_consts()
    cos2, sinS = _rope_tables()
    nch = L // 128
    lg = np.log(1.0 - 2.0 ** (-5.0 - np.arange(4, dtype=np.float32))).astype(np.float32)
    maps = []
    for c in range(NCORES):
        h, dr = c // 2, c % 2

        def prep(a_tm):
            return tm_tiles(a_tm[::-1] if dr else a_tm, nch)

        qh = colsT[64 * h:64 * h + 64].T
        kh = colsT[256 + 64 * h:256 + 64 * h + 64].T
        vh = colsT[512 + 64 * h:512 + 64 * h + 64].T
        sw = lambda a: np.concatenate([a[:, 32:], a[:, :32]], 1)
        maps.append({"tri": tri, "ident": ident, "mneg": mi if dr == 0 else ms,
                     "q": prep(qh), "qsw": prep(sw(qh)), "k": prep(kh), "ksw": prep(sw(kh)), "v": prep(vh),
                     "cq": prep(cos2), "sq": prep(sinS), "ck": prep(cos2 * np.float32(0.125)), "sk": prep(sinS * np.float32(0.125)),
                     "g": np.full((128, nch), lg[h], np.float32)})
    res = run_spmd(build_scan("ret", L), maps)
    return _collect_scan(res)


def _collect_scan(res):
    f = np.zeros((L, 256), np.float32)
    b = np.zeros((L, 256), np.float32)
    for c in range(NCORES):
        h, dr = c // 2, c % 2
        o = un_tm(res[c]["o"])
        if dr:
            b[:, 64 * h:64 * h + 64] = o[::-1]
        else:
            f[:, 64 * h:64 * h + 64] = o
    return f, b


def run_ssd(cs, conv_w, conv_b, a_log, dt_bias, d_skip):
    tri, ident, mi, _, _ = _scan_consts()
    maps = []
    for c in range(NCORES):
        h, dr = c // 2, c % 2
        gi = h // 2
        xr = np.arange(256 + 64 * h, 256 + 64 * h + 64)
        br = np.arange(512 + 128 * gi, 512 + 128 * gi + 128)
        cr = np.arange(768 + 128 * gi, 768 + 128 * gi + 128)

        def cw(rows):
            w = conv_w[:, rows - 256].T
            return np.ascontiguousarray(w[:, ::-1] if dr else w)

        cb = lambda rows: np.ascontiguousarray(conv_b[rows - 256][:, None])
        maps.append({"tri": tri, "ident": ident, "mneg": mi,
                     "xpad": _fm_pad(cs[xr], dr, 2), "bpad": _fm_pad(cs[br], dr, 2), "cpad": _fm_pad(cs[cr], dr, 2),
                     "cwx": cw(xr), "cbx": cb(xr), "cwb": cw(br), "cbb": cb(br), "cwc": cw(cr), "cbc": cb(cr),
                     "dtb": _full(dt_bias[dr, h]), "alog": _full(a_log[dr, h]),
                     "dsk": _full(d_skip[h]) if dr == 0 else np.zeros((128, 1), np.float32),
                     "dtraw": _col_tm(cs[1024 + 4 * dr + h], dr)})
    res = run_spmd(build_scan("ssd", L), maps)
    return _collect_scan(res)


def run_gdn(cg, conv_w, a_log, dt_bias):
    tri, ident, mi, _, pmT = _scan_consts()
    maps = []
    for c in range(NCORES):
        h, dr = c // 2, c % 2
        qr = np.arange(64 * h, 64 * h + 64)

        def cw(rows):
            w = conv_w[:, rows].T
            return np.ascontiguousarray(w[:, ::-1] if dr else w)

        maps.append({"tri": tri, "ident": ident, "mneg": mi, "pmT": pmT,
                     "qpad": _fm_pad(cg[qr], dr, 2), "kpad": _fm_pad(cg[256 + qr], dr, 2), "vpad": _fm_pad(cg[512 + qr], dr, 2),
                     "cwq": cw(qr), "cwk": cw(256 + qr), "cwv": cw(512 + qr),
                     "dtb": _full(dt_bias[dr, h]), "alog": _full(a_log[dr, h]),
                     "araw": _col_tm(cg[1024 + 4 * dr + h], dr), "braw": _col_tm(cg[1032 + 4 * dr + h], dr)})
    res = run_spmd(build_scan("gdn", L), maps)
    return _collect_scan(res)


def kernel(x, p, norm_mix_w, w_in, ret_gn_w, ssm_conv_w, ssm_conv_b, ssm_a_log, ssm_dt_bias,
           ssm_d, ssm_norm_w, hy_conv_w, hy_conv_b, hy_filt_w_in, hy_filt_b_in, hy_filt_w_mid,
           hy_filt_b_mid, hy_filt_freq, hy_filt_w_out, hy_bias, hy_norm_w, gdn_conv_w, gdn_a_log,
           gdn_dt_bias, gdn_norm_w, w_out, norm_ffn_w, router_w, exp_w_gate, exp_w_up, exp_w_down,
           ple_norm_w, ple_gate_w, ple_proj_w, final_norm_w):
    A = lambda a: np.asarray(a, dtype=np.float32)
    hT = np.ascontiguousarray(A(x)[0].T)
    depth = A(w_in).shape[0]
    for i in range(depth):
        maps = [{"hT": np.ascontiguousarray(hT[:, c * TPC:(c + 1) * TPC]), "nw": _colw(A(norm_mix_w)[i]), "w_in": np.ascontiguousarray(A(w_in)[i])}
                for c in range(NCORES)]
        res = run_spmd(build_inproj(), maps)
        colsT = np.concatenate([r["colsT"] for r in res], axis=1)
        c_ret, c_ssm, c_hy, c_gdn = colsT[0:1024], colsT[1024:2056], colsT[2056:2824], colsT[2824:3864]
        ret_f, ret_b = run_ret(c_ret)
        ssd_f, ssd_b = run_ssd(c_ssm, A(ssm_conv_w)[i], A(ssm_conv_b)[i], A(ssm_a_log)[i], A(ssm_dt_bias)[i], A(ssm_d)[i])
        gdn_f, gdn_b = run_gdn(c_gdn, A(gdn_conv_w)[i], A(gdn_a_log)[i], A(gdn_dt_bias)[i])
        hmaps = hyena_inmaps(c_hy, A(hy_conv_w)[i], A(hy_conv_b)[i], A(hy_filt_w_in)[i], A(hy_filt_b_in)[i], A(hy_filt_w_mid)[i],
                             A(hy_filt_b_mid)[i], A(hy_filt_freq)[i], A(hy_filt_w_out)[i], L)
        hy_y, hy_vv, hy_x0 = hyena_collect(run_spmd(build_hyena(L), hmaps), L)
        arr = {"ret_f": ret_f, "ret_b": ret_b, "ssd_f": ssd_f, "ssd_b": ssd_b, "hy_y": hy_y, "hy_vv": hy_vv, "hy_x0": hy_x0,
               "gdn_f": gdn_f, "gdn_b": gdn_b, "g_ret": c_ret[768:1024].T, "z_ssd": c_ssm[0:256].T, "z_gdn": c_gdn[768:1024].T}
        maps = lt_inmaps(hT, arr, A(ret_gn_w)[i], A(ssm_norm_w)[i], A(hy_norm_w)[i], A(hy_bias)[i], A(gdn_norm_w)[i],
                         np.ascontiguousarray(A(w_out)[i]), A(norm_ffn_w)[i], A(router_w)[i])
        h1T, aff = lt_collect(run_spmd(build_lt(), maps))
        final = i == depth - 1
        maps = le_inmaps(h1T, aff, A(norm_ffn_w)[i], A(ple_norm_w)[i], np.ascontiguousarray(A(exp_w_gate)[i]), np.ascontiguousarray(A(exp_w_up)[i]),
                         np.ascontiguousarray(A(exp_w_down)[i]), np.ascontiguousarray(A(ple_gate_w)[i]), np.ascontiguousarray(A(ple_proj_w)[i]),
                         np.ascontiguousarray(A(p)[i, 0].T), A(final_norm_w) if final else None)
        res = run_spmd(build_le(final), maps)
        hT = np.concatenate([r["h3T"] for r in res], axis=1)
    return np.ascontiguousarray(hT.T)[None].astype(np.float32)
```

```python
import contextlib
import os
import numpy as np
import concourse.bass as bass
import concourse.mybir as mybir
from concourse.bass_utils import run_bass_kernel_spmd

F32 = mybir.dt.float32
BF16 = mybir.dt.bfloat16
AF = mybir.ActivationFunctionType
ALU = mybir.AluOpType
AX = mybir.AxisListType

NCORES = 8
DMA_ENGS = ("sync", "pool", "act")
NDSEM = 20


class Prog:
    def __init__(self, name="k"):
        self.nc = bass.Bass("TRN2", target_bir_lowering=False)
        self.stack = contextlib.ExitStack()
        self.engs = ["sync", "act", "dve", "pool", "pe"]
        self.dcnt = {e: 0 for e in DMA_ENGS}
        self.ops = {e: [] for e in self.engs}
        self.cnt = {e: 0 for e in self.engs}
        self.last_w = {}
        self.readers = {}
        self.waited = {e: {} for e in self.engs}
        self.semh = {}
        self.ntiles = 0
        for e in ("act", "dve", "pool", "pe"):
            self.semh[e] = self.stack.enter_context(self.nc.semaphore("s_" + e))
        for e in DMA_ENGS:
            for i in range(NDSEM):
                self.semh[(e, i)] = self.stack.enter_context(self.nc.semaphore("d_%s_%d" % (e, i)))

    def dram(self, name, shape, dtype=F32, kind="Internal"):
        return self.nc.dram_tensor(name, list(shape), dtype, kind=kind).ap()

    def inp(self, name, shape, dtype=F32):
        return self.dram(name, shape, dtype, "ExternalInput")

    def outp(self, name, shape, dtype=F32):
        return self.dram(name, shape, dtype, "ExternalOutput")

    def sbc(self, shape, dtype=F32, name=None):
        if not hasattr(self, "_cache"):
            self._cache = {}
        if name not in self._cache:
            self._cache[name] = self.sb(shape, dtype, name)
        return self._cache[name]

    def sb(self, shape, dtype=F32, name=None):
        self.ntiles += 1
        return self.stack.enter_context(self.nc.sbuf_tensor("sb_" + (name or "t%d" % self.ntiles), list(shape), dtype))

    def ps(self, shape, dtype=F32, name=None):
        self.ntiles += 1
        return self.stack.enter_context(self.nc.psum_tensor("ps_" + (name or "p%d" % self.ntiles), list(shape), dtype))

    def op(self, eng, fn, reads=(), writes=(), dma=False):
        writes = list(writes) + [k for k in reads if isinstance(k, str) and k.startswith("ps")]
        deps = {}

        def add(tok):
            s, v = tok
            if deps.get(s, 0) < v:
                deps[s] = v

        for k in reads:
            if k in self.last_w:
                add(self.last_w[k])
        for k in writes:
            if k in self.last_w:
                add(self.last_w[k])
            for s, v in self.readers.get(k, {}).items():
                add((s, v))
        if dma:
            j = self.dcnt[eng]
            self.dcnt[eng] += 1
            sem = (eng, j % NDSEM)
            val = 16 * (j // NDSEM + 1)
            if j >= NDSEM:
                add((sem, 16 * (j // NDSEM)))
            inc = 16
        else:
            self.cnt[eng] += 1
            sem = eng
            val = self.cnt[eng]
            inc = 1
        waits = []
        for s, v in deps.items():
            if eng == "pe" and s == "pe":
                continue
            if self.waited[eng].get(s, 0) < v:
                waits.append((s, v))
                self.waited[eng][s] = v
        self.ops[eng].append((fn, waits, sem, inc))
        tok = (sem, val)
        for k in reads:
            r = self.readers.setdefault(k, {})
            if r.get(sem, 0) < val:
                r[sem] = val
        for k in writes:
            self.last_w[k] = tok
            self.readers[k] = {}
        return tok

    def i(self, eng, meth, *args, reads=(), writes=(), **kw):
        return self.op(eng, lambda e: getattr(e, meth)(*args, **kw), reads, writes)

    def dma(self, out, in_, reads=(), writes=(), eng="sync", **kw):
        return self.op(eng, lambda e: e.dma_start(out=out, in_=in_, **kw), reads, writes, dma=True)

    def mm(self, out, lhsT, rhs, start=True, stop=True, reads=(), writes=()):
        if getattr(self, "f32r", False) and lhsT.dtype == F32 and rhs.dtype == F32:
            lhsT = lhsT.bitcast(mybir.dt.float32r)
            rhs = rhs.bitcast(mybir.dt.float32r)
        return self.op("pe", lambda e: e.matmul(out, lhsT, rhs, start=start, stop=stop), reads, writes)

    def tr(self, out, in_, ident, reads=(), writes=()):
        return self.op("pe", lambda e: e.transpose(out, in_, ident), reads, writes)

    def act(self, out, in_, func, reads=(), writes=(), **kw):
        return self.op("act", lambda e: e.activation(out=out, in_=in_, func=func, **kw), reads, writes)

    def finish(self):
        for q in DMA_ENGS:
            n = self.dcnt[q]
            waits = []
            for i in range(min(n, NDSEM)):
                last_j = i + NDSEM * ((n - 1 - i) // NDSEM)
                waits.append(((q, i), 16 * (last_j // NDSEM + 1)))
            self.ops[q].append((None, waits, None, 0))
        nc = self.nc
        prog = self

        def run(name, e):
            for fn, waits, sem, inc in prog.ops[name]:
                for s, v in waits:
                    e.wait_ge(prog.semh[s], v)
                if fn is not None:
                    ins = fn(e)
                    ins.then_inc(prog.semh[sem], inc)

        with nc.Block() as block:
            @block.sync
            def _(e):
                run("sync", e)

            @block.scalar
            def _(e):
                run("act", e)

            @block.vector
            def _(e):
                run("dve", e)

            @block.gpsimd
            def _(e):
                run("pool", e)

            @block.tensor
            def _(e):
                run("pe", e)
        self.stack.close()
        return nc


def run_spmd(prog, in_maps):
    nc = prog.finish()
    if os.environ.get("KTRACE"):
        res = run_bass_kernel_spmd(nc, in_maps, core_ids=list(range(NCORES)), trace=True)
        print("KTRACE exec_time_ns", res.exec_time_ns)
    else:
        res = run_bass_kernel_spmd(nc, in_maps, core_ids=list(range(NCORES)))
    return res.results


D = 1024
L = 16384
TPC = L // NCORES
N_IN = 3864


def rmsnorm_fm(P, hT, w_col, out_tiles, ntok, ones, ps_pool, tag, out_keys, h_keys, eps=1e-6):
    nk = len(hT)
    sq = P.sbc([128, 512], F32, name="rms_sq")
    rstd = P.sbc([128, ntok], F32, name="rms_rstd%d" % ntok)
    tag_ = tag
    tag = "rms"
    for n0 in range(0, ntok, 512):
        nn = min(512, ntok - n0)
        pst, psk = ps_pool()
        for k in range(nk):
            P.op("act", lambda e, k=k, n0=n0, nn=nn: e.activation(out=sq[:, :nn], in_=hT[k][:, n0:n0 + nn], func=AF.Square),
                 reads=[h_keys[k]], writes=[tag + "_sq"])
            P.mm(pst[:, :nn], ones[:, :], sq[:, :nn], start=(k == 0), stop=(k == nk - 1), reads=[tag + "_sq", "ones"], writes=[psk])
        P.op("dve", lambda e, n0=n0, nn=nn, pst=pst: e.tensor_scalar(rstd[:, n0:n0 + nn], pst[:, :nn], 1.0 / (128 * nk), eps, op0=ALU.mult, op1=ALU.add),
             reads=[psk], writes=[tag + "_rstd"])
    P.op("act", lambda e: e.activation(out=rstd[:, :], in_=rstd[:, :], func=AF.Sqrt), reads=[tag + "_rstd"], writes=[tag + "_rstd"])
    P.op("dve", lambda e: e.reciprocal(rstd[:, :], rstd[:, :]), reads=[tag + "_rstd"], writes=[tag + "_rstd"])
    for k in range(nk):
        P.op("dve", lambda e, k=k: e.scalar_tensor_tensor(out=out_tiles[k], in0=hT[k], scalar=w_col[:, k:k + 1], in1=rstd[:, :], op0=ALU.mult, op1=ALU.mult),
             reads=[h_keys[k], tag + "_rstd", "wcol_" + tag_], writes=[out_keys[k]])
    return rstd


class PsPool:
    def __init__(self, P, n, shape=(128, 512), dtype=F32, tag="ps"):
        self.t = [(P.ps(list(shape), dtype, name="%s%d" % (tag, i)), "%s%d" % (tag, i)) for i in range(n)]
        self.i = 0

    def __call__(self):
        r = self.t[self.i % len(self.t)]
        self.i += 1
        return r


def build_inproj(layer_tag="l1"):
    P = Prog()
    hT_d = P.inp("hT", [D, TPC])
    nw_d = P.inp("nw", [128, 8])
    win_d = P.inp("w_in", [D, N_IN])
    out_d = P.outp("colsT", [N_IN, TPC])
    ones = P.sb([128, 128], F32, name="ones")
    P.op("dve", lambda e: e.memset(ones[:, :], 1.0), writes=["ones"])
    nw = P.sb([128, 8], F32, name="nw_sb")
    P.dma(nw[:, :], nw_d[:, :], writes=["wcol_n1"])
    psA = PsPool(P, 2, tag="psA")
    psB = PsPool(P, 4, tag="psB")
    wbf = [P.sb([128, N_IN], BF16, name="wbf%d" % k) for k in range(8)]
    wst = [P.sb([128, N_IN], F32, name="wst%d" % i) for i in range(2)]
    for k in range(8):
        st = wst[k % 2]
        P.dma(st[:, :], win_d[k * 128:(k + 1) * 128, :], writes=["wst%d" % (k % 2)], eng="sync" if k % 2 == 0 else "act")
        P.op("pool", lambda e, k=k, st=st: e.tensor_copy(wbf[k][:, :], st[:, :]), reads=["wst%d" % (k % 2)], writes=["wbf%d" % k])
    NT = 1024
    hT = [P.sb([128, NT], F32, name="hT%d" % k) for k in range(8)]
    hn = [P.sb([128, NT], BF16, name="hn%d" % k) for k in range(8)]
    ost = [P.sb([128, 512], F32, name="ost%d" % i) for i in range(4)]
    oi = 0
    for t0 in range(0, TPC, NT):
        for k in range(8):
            P.dma(hT[k][:, :], hT_d[k * 128:(k + 1) * 128, t0:t0 + NT], writes=["hT%d" % k], eng="sync" if k % 2 == 0 else "act")
        rmsnorm_fm(P, [h[:, :] for h in hT], nw, [h[:, :] for h in hn], NT, ones, psA, "n1",
                   ["hn%d" % k for k in range(8)], ["hT%d" % k for k in range(8)])
        for m0 in range(0, N_IN, 128):
            mm_ = min(128, N_IN - m0)
            for n0 in range(0, NT, 512):
                pst, psk = psB()
                for k in range(8):
                    P.mm(pst[:mm_, :], wbf[k][:, m0:m0 + mm_], hn[k][:, n0:n0 + 512], start=(k == 0), stop=(k == 7),
                         reads=["wbf%d" % k, "hn%d" % k], writes=[psk])
                o = ost[oi % 4]
                ok = "ost%d" % (oi % 4)
                if oi % 2 == 0:
                    P.op("act", lambda e, o=o, pst=pst, mm_=mm_: e.copy(o[:mm_, :], pst[:mm_, :]), reads=[psk], writes=[ok])
                else:
                    P.op("dve", lambda e, o=o, pst=pst, mm_=mm_: e.tensor_copy(o[:mm_, :], pst[:mm_, :]), reads=[psk], writes=[ok])
                P.dma(out_d[m0:m0 + mm_, t0 + n0:t0 + n0 + 512], o[:mm_, :], reads=[ok], writes=["out"], eng="sync")
                oi += 1
    return P


import os
SCN = int(os.environ.get("SCN", "16"))
GRP = int(os.environ.get("GRP", "4"))
RD = 2 * GRP + 2
NEG = -1.0e30


class Rot:
    def __init__(self, P, n, shape, dtype, tag):
        self.t = [(P.sb(list(shape), dtype, name="%s%d" % (tag, i)), "%s%d" % (tag, i)) for i in range(n)]
        self.i = 0

    def __call__(self):
        r = self.t[self.i % len(self.t)]
        self.i += 1
        return r


def softplus_tile(P, out, in_, bias_col, shape, tag, rk, wk):
    t = P.sbc(shape, F32, name=tag + "_t")
    a = P.sbc(shape, F32, name=tag + "_a")
    P.op("dve", lambda e: e.tensor_scalar(t[:, :], in_, bias_col, None, op0=ALU.add), reads=rk, writes=[tag + "_t"])
    P.act(a[:, :], t[:, :], AF.Abs, reads=[tag + "_t"], writes=[tag + "_a"])
    P.act(a[:, :], a[:, :], AF.Exp, reads=[tag + "_a"], writes=[tag + "_a"], scale=-1.0)
    P.act(a[:, :], a[:, :], AF.Ln, reads=[tag + "_a"], writes=[tag + "_a"], bias=1.0)
    P.op("dve", lambda e: e.tensor_scalar(t[:, :], t[:, :], 0.0, None, op0=ALU.max), reads=[tag + "_t"], writes=[tag + "_t"])
    P.op("dve", lambda e: e.tensor_tensor(out, t[:, :], a[:, :], op=ALU.add), reads=[tag + "_t", tag + "_a"], writes=wk)


def conv_fm(P, out, xin, w, b, nch, K, width, tag, rk, wk, func):
    acc = P.sbc([128, width], F32, name=tag + "_acc")
    for k in range(K):
        if k == 0:
            P.op("dve", lambda e: e.tensor_scalar(acc[:nch, :], xin[:, 0:width], w[:, 0:1], None, op0=ALU.mult),
                 reads=rk, writes=[tag + "_acc"])
        else:
            P.op("dve", lambda e, k=k: e.scalar_tensor_tensor(out=acc[:nch, :], in0=xin[:, k:k + width], scalar=w[:, k:k + 1], in1=acc[:nch, :], op0=ALU.mult, op1=ALU.add),
                 reads=list(rk) + [tag + "_acc"], writes=[tag + "_acc"])
    if b is None:
        P.act(out, acc[:nch, :], func, reads=[tag + "_acc"], writes=wk)
    else:
        P.act(out, acc[:nch, :], func, reads=[tag + "_acc"], writes=wk, bias=b)


def build_scan(mode, Lx):
    nch = Lx // 128
    nsc = nch // SCN
    W = SCN * 128
    P = Prog()
    P.f32r = bool(int(os.environ.get("F32R", "0")))
    N = 64 if mode in ("ret", "gdn") else 128
    PV = 64
    tri_d = P.inp("tri", [128, 128])
    mneg_d = P.inp("mneg", [128, 128])
    id_d = P.inp("ident", [128, 128])
    out_d = P.outp("o", [128, nch, PV])
    tri = P.sb([128, 128], F32, name="tri")
    mneg = P.sb([128, 128], F32, name="mneg")
    ident = P.sb([128, 128], F32, name="ident")
    ones = P.sb([128, 128], F32, name="ones")
    P.dma(tri[:, :], tri_d[:, :], writes=["tri"])
    P.dma(mneg[:, :], mneg_d[:, :], writes=["mneg"])
    P.dma(ident[:, :], id_d[:, :], writes=["ident"])
    P.op("dve", lambda e: e.memset(ones[:, :], 1.0), writes=["ones"])
    g = P.sb([128, nch], F32, name="g")
    S = P.sb([128, PV], F32, name="S")
    P.op("dve", lambda e: e.memset(S[:, :], 0.0), writes=["S"])

    if mode == "ret":
        names = ["q", "qsw", "k", "ksw", "v", "cq", "sq", "ck", "sk"]
        din = {n: P.inp(n, [128, nch, 64]) for n in names}
        g_d = P.inp("g", [128, nch])
        P.dma(g[:, :], g_d[:, :], writes=["g"])
        tl = {n: [P.sb([128, SCN, 64], F32, name="%s_%d" % (n, i)) for i in range(2)] for n in names}
    elif mode == "ssd":
        xp_d = P.inp("xpad", [64, Lx + 4])
        bp_d = P.inp("bpad", [128, Lx + 4])
        cp_d = P.inp("cpad", [128, Lx + 4])
        prm = {}
        for n, shp in (("cwx", [64, 5]), ("cbx", [64, 1]), ("cwb", [128, 5]), ("cbb", [128, 1]), ("cwc", [128, 5]), ("cbc", [128, 1]),
                       ("dtb", [128, 1]), ("alog", [128, 1]), ("dsk", [128, 1])):
            d = P.inp(n, shp)
            t = P.sb(shp, F32, name=n + "_sb")
            P.dma(t[:, :], d[:, :], writes=[n])
            prm[n] = t
        dtr_d = P.inp("dtraw", [128, nch])
        dtr = P.sb([128, nch], F32, name="dtr")
        dt = P.sb([128, nch], F32, name="dt")
        P.dma(dtr[:, :], dtr_d[:, :], writes=["dtr"])
        softplus_tile(P, dt[:, :], dtr[:, :], prm["dtb"][:, 0:1], [128, nch], "sp", ["dtr", "dtb"], ["dt"])
        ea = P.sb([128, 1], F32, name="ea")
        P.act(ea[:, :], prm["alog"][:, :], AF.Exp, reads=["alog"], writes=["ea"])
        P.op("dve", lambda e: e.tensor_scalar(g[:, :], dt[:, :], ea[:, 0:1], -1.0, op0=ALU.mult, op1=ALU.mult), reads=["dt", "ea"], writes=["g"])
        xin = [P.sb([64, W + 4], F32, name="xin%d" % i) for i in range(2)]
        bin_ = [P.sb([128, W + 4], F32, name="bin%d" % i) for i in range(2)]
        cin = [P.sb([128, W + 4], F32, name="cin%d" % i) for i in range(2)]
        xc = P.sb([64, W], F32, name="xc")
        bc = P.sb([128, W], F32, name="bc")
        cc = P.sb([128, W], F32, name="cc")

    elif mode == "gdn":
        qp_d = P.inp("qpad", [64, Lx + 4])
        kp_d = P.inp("kpad", [64, Lx + 4])
        vp_d = P.inp("vpad", [64, Lx + 4])
        pmT_d = P.inp("pmT", [128, 128])
        pmT = P.sb([128, 128], F32, name="pmT")
        P.dma(pmT[:, :], pmT_d[:, :], writes=["pmT"])
        prm = {}
        for n, shp in (("cwq", [64, 5]), ("cwk", [64, 5]), ("cwv", [64, 5]), ("dtb", [128, 1]), ("alog", [128, 1])):
            d = P.inp(n, shp)
            t = P.sb(shp, F32, name=n + "_sb")
            P.dma(t[:, :], d[:, :], writes=[n])
            prm[n] = t
        ar_d = P.inp("araw", [128, nch])
        br_d = P.inp("braw", [128, nch])
        ar = P.sb([128, nch], F32, name="ar")
        bet = P.sb([128, nch], F32, name="bet")
        dt = P.sb([128, nch], F32, name="dt")
        P.dma(ar[:, :], ar_d[:, :], writes=["ar"])
        P.dma(bet[:, :], br_d[:, :], writes=["bet"])
        P.act(bet[:, :], bet[:, :], AF.Sigmoid, reads=["bet"], writes=["bet"])
        softplus_tile(P, dt[:, :], ar[:, :], prm["dtb"][:, 0:1], [128, nch], "sp", ["ar", "dtb"], ["dt"])
        ea = P.sb([128, 1], F32, name="ea")
        P.act(ea[:, :], prm["alog"][:, :], AF.Exp, reads=["alog"], writes=["ea"])
        P.op("dve", lambda e: e.tensor_scalar(g[:, :], dt[:, :], ea[:, 0:1], -1.0, op0=ALU.mult, op1=ALU.mult), reads=["dt", "ea"], writes=["g"])
        qin = [P.sb([64, W + 4], F32, name="qin%d" % i) for i in range(2)]
        kin = [P.sb([64, W + 4], F32, name="kin%d" % i) for i in range(2)]
        vin = [P.sb([64, W + 4], F32, name="vin%d" % i) for i in range(2)]
        qc = P.sb([64, W], F32, name="qc")
        kc = P.sb([64, W], F32, name="kc")
        vc = P.sb([64, W], F32, name="vc")
        egc = P.sb([128, SCN], F32, name="egc")
        q_t = Rot(P, RD, [128, 64], F32, "q_t")
        k_t = Rot(P, RD, [128, 64], F32, "k_t")
        v_t = Rot(P, RD, [128, 64], F32, "v_t")
        scr = Rot(P, RD, [128, 64], F32, "scr")
        ssq = Rot(P, RD, [128, 2], F32, "ssq")
        kbg = Rot(P, RD, [128, 64], F32, "kbg")
        vbt = Rot(P, RD, [128, 64], F32, "vbt")
        Et = Rot(P, RD, [128, 128], F32, "Et")
        Pm = Rot(P, 3 * GRP, [128, 128], F32, "Pm")
        Qm = Rot(P, 3 * GRP, [128, 128], F32, "Qm")
        Wm = Rot(P, RD, [128, 128], F32, "Wm")
        u_s = Rot(P, RD, [128, 64], F32, "u_s")
        wT_s = Rot(P, RD, [64, 128], F32, "wT_s")
        vn_s = Rot(P, RD, [128, 64], F32, "vn_s")

    psG = PsPool(P, 2, (128, 512), F32, "psG")
    psO = PsPool(P, 1, (128, 512), F32, "psO")
    psU = PsPool(P, 1, (128, 512), F32, "psU")
    psT = PsPool(P, 4, (128, 512), F32, "psT")
    psS = psT
    Gc = P.sb([128, SCN], F32, name="Gc")
    ngc = P.sb([128, SCN], F32, name="ngc")
    gb = Rot(P, RD, [128, 128], F32, "gb")
    Dm = Rot(P, RD, [128, 128], F32, "Dm")
    AT = Rot(P, RD, [128, 128], F32, "AT")
    EG = Rot(P, RD, [128, 128], F32, "EG")
    qd = Rot(P, RD, [128, 128], F32, "qd")
    qTs = Rot(P, RD, [128, 128], F32, "qTs")
    kTs = Rot(P, RD, [128, 128], F32, "kTs")
    ktm = Rot(P, RD, [128, 128], F32, "ktm")
    vtm = Rot(P, RD, [128, PV], F32, "vtm")
    xtm = Rot(P, RD, [128, PV], F32, "xtm")
    kd = Rot(P, RD, [128, 128], F32, "kd")
    gl = Rot(P, RD, [128, 4], F32, "gl")
    osb = [P.sb([128, SCN, PV], F32, name="osb%d" % i) for i in range(2)]

    for s in range(nsc):
        b = s % 2
        if mode == "ret":
            for i, n in enumerate(names):
                P.dma(tl[n][b][:, :, :], din[n][:, s * SCN:(s + 1) * SCN, :], writes=["%s_%d" % (n, b)], eng="sync")
            for (x, xs, c_, s_) in (("q", "qsw", "cq", "sq"), ("k", "ksw", "ck", "sk")):
                kx, kxs, kc, ks = ["%s_%d" % (n, b) for n in (x, xs, c_, s_)]
                P.op("dve", lambda e, x=x, c_=c_, b=b: e.tensor_tensor(tl[x][b][:, :, :], tl[x][b][:, :, :], tl[c_][b][:, :, :], op=ALU.mult), reads=[kx, kc], writes=[kx])
                P.op("pool", lambda e, xs=xs, s_=s_, b=b: e.tensor_tensor(tl[xs][b][:, :, :], tl[xs][b][:, :, :], tl[s_][b][:, :, :], op=ALU.mult), reads=[kxs, ks], writes=[kxs])
                P.op("dve", lambda e, x=x, xs=xs, b=b: e.tensor_tensor(tl[x][b][:, :, :], tl[x][b][:, :, :], tl[xs][b][:, :, :], op=ALU.add), reads=[kx, kxs], writes=[kx])
        elif mode == "ssd":
            P.dma(xin[b][:, :], xp_d[:, s * W:s * W + W + 4], writes=["xin%d" % b])
            P.dma(bin_[b][:, :], bp_d[:, s * W:s * W + W + 4], writes=["bin%d" % b], eng="sync")
            P.dma(cin[b][:, :], cp_d[:, s * W:s * W + W + 4], writes=["cin%d" % b])
            conv_fm(P, xc[:, :], xin[b], prm["cwx"], prm["cbx"][:, 0:1], 64, 5, W, "cv", ["xin%d" % b, "cwx"], ["xc"], AF.Silu)
            conv_fm(P, bc[:, :], bin_[b], prm["cwb"], prm["cbb"][:, 0:1], 128, 5, W, "cv", ["bin%d" % b, "cwb"], ["bc"], AF.Silu)
            conv_fm(P, cc[:, :], cin[b], prm["cwc"], prm["cbc"][:, 0:1], 128, 5, W, "cv", ["cin%d" % b, "cwc"], ["cc"], AF.Silu)
        elif mode == "gdn":
            P.dma(qin[b][:, :], qp_d[:, s * W:s * W + W + 4], writes=["qin%d" % b])
            P.dma(kin[b][:, :], kp_d[:, s * W:s * W + W + 4], writes=["kin%d" % b], eng="sync")
            P.dma(vin[b][:, :], vp_d[:, s * W:s * W + W + 4], writes=["vin%d" % b])
            conv_fm(P, qc[:, :], qin[b], prm["cwq"], None, 64, 5, W, "cv", ["qin%d" % b, "cwq"], ["qc"], AF.Silu)
            conv_fm(P, kc[:, :], kin[b], prm["cwk"], None, 64, 5, W, "cv", ["kin%d" % b, "cwk"], ["kc"], AF.Silu)
            conv_fm(P, vc[:, :], vin[b], prm["cwv"], None, 64, 5, W, "cv", ["vin%d" % b, "cwv"], ["vc"], AF.Silu)
        pt, pk = psT()
        P.mm(pt[:, :SCN], tri[:, :], g[:, s * SCN:(s + 1) * SCN], reads=["tri", "g"], writes=[pk])
        P.op("dve", lambda e, pt=pt: e.tensor_copy(Gc[:, :], pt[:, :SCN]), reads=[pk], writes=["Gc"])
        if mode == "gdn":
            P.act(egc[:, :], Gc[:, :], AF.Exp, reads=["Gc"], writes=["egc"])

        def chunk_gen(s, b, c):
                cg = s * SCN + c
                qT, qTk = qTs()
                kT, kTk = kTs()
                if mode == "ret":
                    pt, pk = psT()
                    P.tr(pt[:64, :128], tl["q"][b][:, c, :], ident[:, :], reads=["q_%d" % b, "ident"], writes=[pk])
                    P.act(qT[:64, :], pt[:64, :128], AF.Copy, reads=[pk], writes=[qTk])
                    yield
                    pt, pk = psT()
                    P.tr(pt[:64, :128], tl["k"][b][:, c, :], ident[:, :], reads=["k_%d" % b, "ident"], writes=[pk])
                    P.act(kT[:64, :], pt[:64, :128], AF.Copy, reads=[pk], writes=[kTk])
                    yield
                    qT_ap, kT_ap = qT[:64, :], kT[:64, :]
                    k_tm_ap = tl["k"][b][:, c, :]
                    v_ap = tl["v"][b][:, c, :]
                    rq, rk_, rktm, rv = [qTk], [kTk], ["k_%d" % b], ["v_%d" % b]
                elif mode == "ssd":
                    qT_ap, kT_ap = cc[:, c * 128:(c + 1) * 128], bc[:, c * 128:(c + 1) * 128]
                    rq, rk_ = ["cc"], ["bc"]
                    kt, ktk = ktm()
                    pt, pk = psT()
                    P.tr(pt[:, :128], bc[:, c * 128:(c + 1) * 128], ident[:, :], reads=["bc", "ident"], writes=[pk])
                    P.act(kt[:, :], pt[:, :128], AF.Copy, reads=[pk], writes=[ktk])
                    yield
                    xt, xtk = xtm()
                    vt, vtk = vtm()
                    pt, pk = psT()
                    P.tr(pt[:, :64], xc[:, c * 128:(c + 1) * 128], ident[:64, :64], reads=["xc", "ident"], writes=[pk])
                    P.act(xt[:, :], pt[:, :64], AF.Copy, reads=[pk], writes=[xtk])
                    yield
                    P.op("dve", lambda e, vt=vt, xt=xt, cg=cg: e.tensor_scalar(vt[:, :], xt[:, :], dt[:, cg:cg + 1], None, op0=ALU.mult), reads=[xtk, "dt"], writes=[vtk])
                    k_tm_ap, v_ap = kt[:, :], vt[:, :]
                    rktm, rv = [ktk], [vtk]
                elif mode == "gdn":
                    tms = []
                    for (src, srck, rot) in ((qc, "qc", q_t), (kc, "kc", k_t), (vc, "vc", v_t)):
                        tt, ttk = rot()
                        pt, pk = psT()
                        P.tr(pt[:, :64], src[:, c * 128:(c + 1) * 128], ident[:64, :64], reads=[srck, "ident"], writes=[pk])
                        P.act(tt[:, :], pt[:, :64], AF.Copy, reads=[pk], writes=[ttk])
                        yield
                        tms.append((tt, ttk))
                    (qt_, qtk), (kt_, ktk_), (vt_, vtk_) = tms
                    sc_, sck = scr()
                    ss_, ssk = ssq()
                    P.act(sc_[:, :], qt_[:, :], AF.Square, reads=[qtk], writes=[sck, ssk], accum_out=ss_[:, 0:1])
                    P.act(sc_[:, :], kt_[:, :], AF.Square, reads=[ktk_], writes=[sck, ssk], accum_out=ss_[:, 1:2])
                    P.op("dve", lambda e, ss_=ss_: e.tensor_scalar(ss_[:, :], ss_[:, :], 1e-6, None, op0=ALU.add), reads=[ssk], writes=[ssk])
                    P.act(ss_[:, :], ss_[:, :], AF.Sqrt, reads=[ssk], writes=[ssk])
                    P.op("dve", lambda e, ss_=ss_: e.reciprocal(ss_[:, :], ss_[:, :]), reads=[ssk], writes=[ssk])
                    P.op("dve", lambda e, qt_=qt_, ss_=ss_: e.tensor_scalar(qt_[:, :], qt_[:, :], ss_[:, 0:1], 0.125, op0=ALU.mult, op1=ALU.mult), reads=[qtk, ssk], writes=[qtk])
                    P.op("dve", lambda e, kt_=kt_, ss_=ss_: e.tensor_scalar(kt_[:, :], kt_[:, :], ss_[:, 1:2], None, op0=ALU.mult), reads=[ktk_, ssk], writes=[ktk_])
                    pt, pk = psT()
                    P.tr(pt[:64, :128], qt_[:, :], ident[:, :], reads=[qtk, "ident"], writes=[pk])
                    P.act(qT[:64, :], pt[:64, :128], AF.Copy, reads=[pk], writes=[qTk])
                    yield
                    pt, pk = psT()
                    P.tr(pt[:64, :128], kt_[:, :], ident[:, :], reads=[ktk_, "ident"], writes=[pk])
                    P.act(kT[:64, :], pt[:64, :128], AF.Copy, reads=[pk], writes=[kTk])
                    yield
                    qT_ap, kT_ap = qT[:64, :], kT[:64, :]
                    k_tm_ap = kt_[:, :]
                    rq, rk_, rktm = [qTk], [kTk], [ktk_]
                    kb_, kbk = kbg()
                    vb_, vbk = vbt()
                    P.op("dve", lambda e, kb_=kb_, kt_=kt_, cg=cg, c=c: e.tensor_scalar(kb_[:, :], kt_[:, :], bet[:, cg:cg + 1], egc[:, c:c + 1], op0=ALU.mult, op1=ALU.mult),
                         reads=[ktk_, "bet", "egc"], writes=[kbk])
                    P.op("pool", lambda e, vb_=vb_, vt_=vt_, cg=cg: e.tensor_scalar(vb_[:, :], vt_[:, :], bet[:, cg:cg + 1], None, op0=ALU.mult), reads=[vtk_, "bet"], writes=[vbk])
                gbt, gbk = gb()
                P.op("pool", lambda e, gbt=gbt, cg=cg: e.tensor_scalar(gbt[:, :], ones[:, :], g[:, cg:cg + 1], None, op0=ALU.mult), reads=["ones", "g"], writes=[gbk])
                pG, pGk = psG()
                P.mm(pG[:, :128], gbt[:, :], tri[:, :], reads=[gbk, "tri"], writes=[pGk])
                dm, dmk = Dm()
                P.op("dve", lambda e, dm=dm, pG=pG, c=c: e.scalar_tensor_tensor(out=dm[:, :], in0=pG[:, :128], scalar=Gc[:, c:c + 1], in1=mneg[:, :], op0=ALU.subtract, op1=ALU.add),
                     reads=[pGk, "Gc", "mneg"], writes=[dmk])
                P.act(dm[:, :], dm[:, :], AF.Exp, reads=[dmk], writes=[dmk])
                eg, egk = EG()
                P.act(eg[:N, :], pG[:N, :128], AF.Exp, reads=[pGk], writes=[egk])
                glt, glk = gl()
                P.op("dve", lambda e, glt=glt, pG=pG: e.tensor_copy(glt[:, 0:1], pG[:, 127:128]), reads=[pGk], writes=[glk])
                P.act(glt[:, 1:2], glt[:, 0:1], AF.Exp, reads=[glk], writes=[glk])
                P.act(glt[:, 2:3], Gc[:, c:c + 1], AF.Exp, reads=[glk, "Gc"], writes=[glk], scale=-1.0, bias=glt[:, 0:1])
                if mode == "gdn":
                    et, etk = Et()
                    P.op("dve", lambda e, et=et, pG=pG, c=c: e.scalar_tensor_tensor(out=et[:, :], in0=pG[:, :128], scalar=Gc[:, c:c + 1], in1=pmT[:, :], op0=ALU.subtract, op1=ALU.add),
                         reads=[pGk, "Gc", "pmT"], writes=[etk])
                    P.act(et[:, :], et[:, :], AF.Exp, reads=[etk], writes=[etk], scale=-1.0)
                    pK, pKk = psT()
                    P.mm(pK[:, :128], kT_ap, kT_ap, reads=rk_, writes=[pKk])
                    Qc, Qk = Qm()
                    P.op("dve", lambda e, Qc=Qc, pK=pK, et=et, cg=cg: e.scalar_tensor_tensor(out=Qc[:, :], in0=pK[:, :128], scalar=bet[:, cg:cg + 1], in1=et[:, :], op0=ALU.mult, op1=ALU.mult),
                         reads=[pKk, "bet", etk], writes=[Qk])
                    yield
                    pM, pMk = psT()
                    P.tr(pM[:, :128], Qc[:, :], ident[:, :], reads=[Qk, "ident"], writes=[pMk])
                    Pc, Pk = Pm()
                    P.act(Pc[:, :], pM[:, :128], AF.Copy, reads=[pMk], writes=[Pk])
                    Wc, Wk = Wm()
                    P.op("dve", lambda e, Wc=Wc, pM=pM: e.tensor_tensor(Wc[:, :], ident[:, :], pM[:, :128], op=ALU.subtract), reads=["ident", pMk], writes=[Wk])
                    for lev in range(1, 7):
                        pQ, pQk = psT()
                        P.mm(pQ[:, :128], Pc[:, :], Qc[:, :], reads=[Pk, Qk], writes=[pQk])
                        if lev < 6:
                            pP, pPk = psT()
                            P.mm(pP[:, :128], Qc[:, :], Pc[:, :], reads=[Pk, Qk], writes=[pPk])
                        Qn, Qnk = Qm()
                        P.act(Qn[:, :], pQ[:, :128], AF.Copy, reads=[pQk], writes=[Qnk])
                        if lev < 6:
                            Pn, Pnk = Pm()
                            P.op("dve", lambda e, Pn=Pn, pP=pP: e.tensor_copy(Pn[:, :], pP[:, :128]), reads=[pPk], writes=[Pnk])
                            yield
                            Pc, Pk = Pn, Pnk
                        Qc, Qk = Qn, Qnk
                        pW, pWk = psT()
                        P.mm(pW[:, :128], Qc[:, :], Wc[:, :], reads=[Qk, Wk], writes=[pWk])
                        P.op("dve", lambda e, Wc=Wc, pW=pW: e.tensor_tensor(Wc[:, :], Wc[:, :], pW[:, :128], op=ALU.add), reads=[Wk, pWk], writes=[Wk])
                        yield
                    pu, puk = psT()
                    P.mm(pu[:, :64], Wc[:, :], vb_[:, :], reads=[Wk, vbk], writes=[puk])
                    us, usk = u_s()
                    P.act(us[:, :], pu[:, :64], AF.Copy, reads=[puk], writes=[usk])
                    yield
                    pw, pwk = psT()
                    P.mm(pw[:64, :128], kb_[:, :], Wc[:, :], reads=[kbk, Wk], writes=[pwk])
                    ws, wsk = wT_s()
                    P.act(ws[:, :], pw[:64, :128], AF.Copy, reads=[pwk], writes=[wsk])
                    yield
                pS, pSk = psS()
                P.mm(pS[:, :128], kT_ap, qT_ap, reads=rk_ + rq, writes=[pSk])
                at, atk = AT()
                P.op("dve", lambda e, at=at, pS=pS, dm=dm: e.tensor_tensor(at[:, :], pS[:, :128], dm[:, :], op=ALU.mult), reads=[pSk, dmk], writes=[atk])
                yield
                qdt, qdk = qd()
                P.op("pool", lambda e, qdt=qdt, qT_ap=qT_ap, eg=eg: e.tensor_tensor(qdt[:N, :], qT_ap, eg[:N, :], op=ALU.mult), reads=rq + [egk], writes=[qdk])
                kdt, kdk = kd()
                P.op("dve", lambda e, kdt=kdt, k_tm_ap=k_tm_ap, glt=glt: e.tensor_scalar(kdt[:, :N], k_tm_ap, glt[:, 2:3], None, op0=ALU.mult), reads=rktm + [glk], writes=[kdk])
                yield "B"
                if mode == "gdn":
                    pv, pvk = psT()
                    P.mm(pv[:, :64], ws[:, :], S[:64, :], reads=[wsk, "S"], writes=[pvk])
                    vn, vnk = vn_s()
                    P.op("dve", lambda e, vn=vn, us=us, pv=pv: e.tensor_tensor(vn[:, :], us[:, :], pv[:, :64], op=ALU.subtract), reads=[usk, pvk], writes=[vnk])
                    yield
                    v_ap = vn[:, :]
                    rv = [vnk]
                pO, pOk = psO()
                P.mm(pO[:, :PV], at[:, :], v_ap, start=True, stop=False, reads=[atk] + rv, writes=[pOk])
                P.mm(pO[:, :PV], qdt[:N, :], S[:N, :], start=False, stop=True, reads=[qdk, "S"], writes=[pOk])
                if mode == "ssd":
                    P.op("dve", lambda e, pO=pO, xt=xt, c=c, b=b: e.scalar_tensor_tensor(out=osb[b][:, c, :], in0=xt[:, :], scalar=prm["dsk"][:, 0:1], in1=pO[:, :PV], op0=ALU.mult, op1=ALU.add),
                         reads=[pOk, xtk, "dsk"], writes=["osb%d" % b])
                    yield
                else:
                    P.act(osb[b][:, c, :], pO[:, :PV], AF.Copy, reads=[pOk], writes=["osb%d" % b])
                    yield
                pU, pUk = psU()
                P.mm(pU[:N, :PV], kdt[:, :N], v_ap, reads=[kdk] + rv, writes=[pUk])
                P.op("dve", lambda e, pU=pU, glt=glt: e.scalar_tensor_tensor(out=S[:N, :], in0=S[:N, :], scalar=glt[:N, 1:2], in1=pU[:N, :PV], op0=ALU.mult, op1=ALU.add),
                     reads=["S", glk, pUk], writes=["S"])
                yield

        gens = [chunk_gen(s, b, c) for c in range(SCN)]
        gi, active, waitb = 0, [], []
        while gi < SCN or active or waitb:
            while len(active) < GRP and len(active) + len(waitb) < 2 * GRP and gi < SCN:
                active.append(gens[gi])
                gi += 1
            for gq in list(active):
                r = next(gq, "DONE")
                if r == "B":
                    active.remove(gq)
                    waitb.append(gq)
                elif r == "DONE":
                    active.remove(gq)
            if waitb:
                r = next(waitb[0], "DONE")
                if r == "DONE":
                    waitb.pop(0)
        P.dma(out_d[:, s * SCN:(s + 1) * SCN, :], osb[b][:, :, :], reads=["osb%d" % b], writes=["out"], eng="pool")
    return P


TWO_PI = 6.283185307179586
MAGIC = 12582912.0


def sinred(P, out, ps_ap, freq, fb, nrow, ncol, tag, rk, wk):
    y = P.sbc([128, 512], F32, name=tag + "_y")
    kf = P.sbc([128, 512], F32, name=tag + "_k")
    yk, kk = tag + "_y", tag + "_k"
    P.i("dve", "tensor_scalar", y[:nrow, :ncol], ps_ap, freq, fb, op0=ALU.mult, op1=ALU.add, reads=rk, writes=[yk])
    P.i("dve", "tensor_scalar", kf[:nrow, :ncol], y[:nrow, :ncol], 1.0 / TWO_PI, MAGIC, op0=ALU.mult, op1=ALU.add, reads=[yk], writes=[kk])
    P.i("dve", "tensor_scalar", kf[:nrow, :ncol], kf[:nrow, :ncol], -MAGIC, None, op0=ALU.add, reads=[kk], writes=[kk])
    P.i("dve", "scalar_tensor_tensor", out=y[:nrow, :ncol], in0=kf[:nrow, :ncol], scalar=-TWO_PI, in1=y[:nrow, :ncol], op0=ALU.mult, op1=ALU.add, reads=[yk, kk], writes=[yk])
    P.i("dve", "tensor_scalar", y[:nrow, :ncol], y[:nrow, :ncol], 3.1415925, -3.1415925, op0=ALU.min, op1=ALU.max, reads=[yk], writes=[yk])
    P.act(out, y[:nrow, :ncol], AF.Sin, reads=[yk], writes=wk)


def sinred_gen(P, out, ps_ap, freq, fb, nrow, ncol, tag, rk, wk):
    y = P.sbc([128, 512], F32, name=tag + "_y")
    kf = P.sbc([128, 512], F32, name=tag + "_k")
    yk, kk = tag + "_y", tag + "_k"
    P.i("dve", "tensor_scalar", y[:nrow, :ncol], ps_ap, freq, fb, op0=ALU.mult, op1=ALU.add, reads=rk, writes=[yk])
    yield
    P.i("dve", "tensor_scalar", kf[:nrow, :ncol], y[:nrow, :ncol], 1.0 / TWO_PI, MAGIC, op0=ALU.mult, op1=ALU.add, reads=[yk], writes=[kk])
    P.i("dve", "tensor_scalar", kf[:nrow, :ncol], kf[:nrow, :ncol], -MAGIC, None, op0=ALU.add, reads=[kk], writes=[kk])
    yield
    P.i("dve", "scalar_tensor_tensor", out=y[:nrow, :ncol], in0=kf[:nrow, :ncol], scalar=-TWO_PI, in1=y[:nrow, :ncol], op0=ALU.mult, op1=ALU.add, reads=[yk, kk], writes=[yk])
    P.i("dve", "tensor_scalar", y[:nrow, :ncol], y[:nrow, :ncol], 3.1415925, -3.1415925, op0=ALU.min, op1=ALU.max, reads=[yk], writes=[yk])
    yield
    P.act(out, y[:nrow, :ncol], AF.Sin, reads=[yk], writes=wk)
    yield


def build_hyena(Lx):
    nb = Lx // 128
    P = Prog()
    nc = P.nc
    CH = 32
    ins = {}
    for n, shp in (("x0pad", [CH, Lx + 2]), ("x1pad", [CH, Lx + 2]), ("vpad", [CH, Lx + 2]), ("zT", [33, 2 * Lx]), ("win", [CH, 2 * Lx]),
                   ("ident", [128, 128])):
        ins[n] = P.inp(n, shp)
    prm = {}
    for n, shp in (("cw0", [CH, 3]), ("cw1", [CH, 3]), ("cwv", [CH, 3]), ("cb0", [CH, 1]), ("cb1", [CH, 1]), ("cbv", [CH, 1]),
                   ("w_in", [33, 64]), ("b_in", [64, 1]), ("w_mid0", [64, 64]), ("w_mid1", [64, 64]), ("b_mid0", [64, 1]), ("b_mid1", [64, 1]),
                   ("freq", [64, 1]), ("w_out_f", [64, CH]), ("w_out_b", [64, CH])):
        d = P.inp(n, shp)
        t = P.sb(shp, F32, name=n + "_sb")
        P.dma(t[:, :], d[:, :], writes=[n])
        prm[n] = t
    yr_d = P.outp("yr", [128, CH, nb])
    vv_d = P.outp("vv", [CH, Lx])
    x0_d = P.outp("x0c", [CH, Lx])
    fr_d = P.dram("fr_scratch", [CH, 2 * Lx], BF16)
    ident = P.sb([128, 128], F32, name="ident")
    P.dma(ident[:, :], ins["ident"][:, :], writes=["ident"])
    fb = P.sb([64, 3], F32, name="fb")
    for i, bn in enumerate(("b_in", "b_mid0", "b_mid1")):
        P.i("dve", "tensor_tensor", fb[:, i:i + 1], prm[bn][:, :], prm["freq"][:, :], op=ALU.mult, reads=[bn, "freq"], writes=["fb"])
    psM = PsPool(P, 4, (128, 512), F32, "psM")
    psY = PsPool(P, 2, (128, 512), F32, "psY")
    psT = PsPool(P, 2, (128, 512), F32, "psT")
    NQ = 2
    NQ = 2
    Vt = P.sb([128, CH, nb], BF16, name="Vt")
    SW = min(2048, Lx)
    xin = {n: Rot(P, 1, [CH, SW + 2], F32, n + "in") for n in ("x0pad", "x1pad", "vpad")}
    x0c = Rot(P, 1, [CH, SW], F32, "x0c")
    x1c = P.sb([CH, SW], F32, name="x1c")
    vc = Rot(P, 1, [CH, SW], F32, "vc")
    for s0 in range(0, Lx, SW):
        tl = {}
        for i, n in enumerate(("x0pad", "x1pad", "vpad")):
            t, tk = xin[n]()
            P.dma(t[:, :], ins[n][:, s0:s0 + SW + 2], writes=[tk], eng="sync")
            tl[n] = (t, tk)
        a0, a0k = x0c()
        v_, vk = vc()
        conv_fm(P, a0[:, :], tl["x0pad"][0], prm["cw0"], prm["cb0"][:, 0:1], CH, 3, SW, "hc", [tl["x0pad"][1], "cw0"], [a0k], AF.Identity)
        conv_fm(P, x1c[:, :], tl["x1pad"][0], prm["cw1"], prm["cb1"][:, 0:1], CH, 3, SW, "hc", [tl["x1pad"][1], "cw1"], ["x1c"], AF.Identity)
        conv_fm(P, v_[:, :], tl["vpad"][0], prm["cwv"], prm["cbv"][:, 0:1], CH, 3, SW, "hc", [tl["vpad"][1], "cwv"], [vk], AF.Identity)
        P.i("dve", "tensor_tensor", v_[:, :], v_[:, :], x1c[:, :], op=ALU.mult, reads=[vk, "x1c"], writes=[vk])
        P.dma(vv_d[:, s0:s0 + SW], v_[:, :], reads=[vk], writes=["vv_d"])
        P.dma(x0_d[:, s0:s0 + SW], a0[:, :], reads=[a0k], writes=["x0_d"], eng="sync")
        for c in range(SW // 128):
            pt, pk = psT()
            P.tr(pt[:, :CH], v_[:, c * 128:(c + 1) * 128], ident[:CH, :CH], reads=[vk, "ident"], writes=[pk])
            P.act(Vt[:, :, s0 // 128 + c], pt[:, :CH], AF.Copy, reads=[pk], writes=["Vt"])
    NFG = 3
    zt = Rot(P, NFG + 1, [33, 512], F32, "zt")
    wn = Rot(P, NFG + 1, [CH, 512], F32, "wn")
    hs = [Rot(P, NFG + 1, [64, 512], F32, "h%d" % i) for i in range(3)]
    fo = Rot(P, NFG + 1, [CH, 512], BF16, "fo")
    fq = prm["freq"][:, 0:1]

    def fgen(x0, slot):
        tag = "sr%d" % slot
        z, zk = zt()
        w_, wk_ = wn()
        P.dma(z[:, :], ins["zT"][:, x0:x0 + 512], writes=[zk])
        P.dma(w_[:, :], ins["win"][:, x0:x0 + 512], writes=[wk_], eng="sync")
        hprev, hprevk = z, zk
        for li, (wname, kdim) in enumerate((("w_in", 33), ("w_mid0", 64), ("w_mid1", 64))):
            p, pk = psM()
            P.mm(p[:64, :], prm[wname][:, :], hprev[:, :], reads=[wname, hprevk], writes=[pk])
            hcur, hcurk = hs[li]()
            for _ in sinred_gen(P, hcur[:, :], p[:64, :], fq, fb[:, li:li + 1], 64, 512, tag, [pk, "freq", "fb"], [hcurk]):
                yield
            hprev, hprevk = hcur, hcurk
        p, pk = psM()
        wsel = "w_out_f" if x0 < Lx else "w_out_b"
        P.mm(p[:CH, :], prm[wsel][:, :], hprev[:, :], reads=[wsel, hprevk], writes=[pk])
        f_, fk = fo()
        P.i("dve", "tensor_tensor", f_[:, :], p[:CH, :], w_[:, :], op=ALU.mult, reads=[pk, wk_], writes=[fk])
        yield
        P.dma(fr_d[:, x0:x0 + 512], f_[:, :], reads=[fk], writes=["fr_q1h" if x0 == Lx else "fr_q%d" % (x0 // (2 * Lx // NQ))])

    def run_fg(x0s, extra=None):
        xi = 0
        active = []
        free_slots = list(range(NFG))
        while xi < len(x0s) or active:
            while free_slots and xi < len(x0s):
                sl = free_slots.pop(0)
                active.append((fgen(x0s[xi], sl), sl))
                xi += 1
            for item in list(active):
                gq, sl = item
                if next(gq, "DONE") == "DONE":
                    active.remove(item)
                    free_slots.append(sl)
            if extra is not None:
                next(extra, None)
        if extra is not None:
            for _ in extra:
                pass

    QW = 2 * Lx // NQ
    assert NQ == 2
    FS = Rot(P, 2, [128, QW], BF16, "FS")
    yst = P.sb([128, CH, nb], F32, name="yst")
    NSPLIT = 4

    def main_q(q):
        for ch in range(CH):
            pY, pYk = psY()
            fs, fsk = FS()
            cw = QW // NSPLIT
            for sp in range(NSPLIT):
                c0 = sp * cw
                ncol = cw
                if q == NQ - 1:
                    ncol = min(ncol, QW - 127 - c0)
                src = bass.AP(fr_d.tensor, ch * 2 * Lx + q * QW + c0, [[1, 128], [1, ncol]])
                rk = ["fr_q%d" % q] + (["fr_q1h"] if (q == NQ - 1 or sp == NSPLIT - 1) else [])
                P.dma(fs[:, c0:c0 + ncol], src, reads=rk, writes=[fsk], eng=("sync", "pool")[sp % 2])
            ds = []
            for off in range(0, QW, 128):
                base2 = q * QW + off
                d = (Lx - base2) // 128 - 1
                if -nb < d < nb:
                    ds.append((d, off))
            ds.sort(key=lambda t: abs(t[0]))
            first = True
            for n_, (d, off) in enumerate(ds):
                a_lo, a_hi = max(0, d), min(nb, nb + d)
                P.mm(pY[:, a_lo:a_hi], fs[:, off:off + 128], Vt[:, ch, a_lo - d:a_hi - d], start=first, stop=False,
                     reads=[fsk, "Vt"], writes=[pYk])
                first = False
                if n_ % 16 == 15:
                    yield
            if q == 0:
                P.act(yst[:, ch, :], pY[:, :nb], AF.Copy, reads=[pYk], writes=["yst"])
            else:
                P.i("dve", "tensor_tensor", yst[:, ch, 0:nb - 1], yst[:, ch, 0:nb - 1], pY[:, 0:nb - 1], op=ALU.add, reads=[pYk, "yst"], writes=["yst"])
            yield

    x0s = list(range(0, 2 * Lx, 512))
    run_fg([x for x in x0s if x < Lx])
    run_fg([Lx])
    run_fg([x for x in x0s if x > Lx], extra=main_q(0))
    for _ in main_q(1):
        pass
    P.dma(yr_d[:, :, :], yst[:, :, :], reads=["yst"], writes=["yr_d"])
    return P


def hyena_consts(Lx):
    f32 = np.float32
    t = np.linspace(0.0, 1.0, Lx, dtype=f32)[:, None]
    bands = np.linspace(1e-4, 15.0, 16, dtype=f32)
    ang = (f32(2.0 * np.pi / Lx) * np.arange(Lx, dtype=f32)[:, None] * bands[None]).astype(f32)
    z = np.concatenate([t, np.cos(ang), -np.sin(ang)], axis=-1).astype(f32)
    max_decay = np.log(1e-2) / 0.3
    min_decay = np.log(1e-2) / 1.5
    deltas = np.abs(np.linspace(min_decay, max_decay, 256, dtype=f32))
    win = (np.exp(-t * deltas[None]) + f32(0.05)).astype(f32)
    m = np.concatenate([Lx - 1 - np.arange(Lx), np.minimum(np.arange(Lx) + 1, Lx - 1)])
    zT = np.ascontiguousarray(z[m].T)
    winx = win[m].copy()
    winx[2 * Lx - 1] = 0.0
    return zT, np.ascontiguousarray(winx.T)


def hyena_inmaps(cT, conv_w, conv_b, w_in, b_in, w_mid, b_mid, freq, w_out, Lx):
    zT, winx = hyena_consts(Lx)
    ident = np.eye(128, dtype=np.float32)
    maps = []
    col = lambda a: np.ascontiguousarray(a.reshape(-1, 1))
    for c in range(NCORES):
        ch = np.arange(32 * c, 32 * c + 32)
        pad = lambda rows: np.ascontiguousarray(np.pad(cT[rows], ((0, 0), (1, 1))))
        m = {"x0pad": pad(ch), "x1pad": pad(256 + ch), "vpad": pad(512 + ch), "zT": zT, "win": np.ascontiguousarray(winx[ch]), "ident": ident,
             "cw0": np.ascontiguousarray(conv_w[:, ch].T), "cw1": np.ascontiguousarray(conv_w[:, 256 + ch].T), "cwv": np.ascontiguousarray(conv_w[:, 512 + ch].T),
             "cb0": col(conv_b[ch]), "cb1": col(conv_b[256 + ch]), "cbv": col(conv_b[512 + ch]),
             "w_in": np.ascontiguousarray(w_in), "b_in": col(b_in), "w_mid0": np.ascontiguousarray(w_mid[0]), "w_mid1": np.ascontiguousarray(w_mid[1]),
             "b_mid0": col(b_mid[0]), "b_mid1": col(b_mid[1]), "freq": col(freq),
             "w_out_f": np.ascontiguousarray(w_out[:, ch]), "w_out_b": np.ascontiguousarray(w_out[:, 256 + ch])}
        maps.append(m)
    return maps


def hyena_collect(res, Lx):
    nb = Lx // 128
    ys, vs, xs = [], [], []
    for c in range(NCORES):
        yr = res[c]["yr"]
        y = yr[::-1].transpose(2, 0, 1).reshape(Lx, 32)
        ys.append(y)
        vs.append(res[c]["vv"].T)
        xs.append(res[c]["x0c"].T)
    return np.concatenate(ys, 1), np.concatenate(vs, 1), np.concatenate(xs, 1)


def bc3(ap2, n):
    return ap2.unsqueeze(2).to_broadcast([ap2.shape[0], ap2.shape[1], n])


def rstd_small(P, t, key, scale, eps=1e-6):
    P.i("dve", "tensor_scalar", t, t, scale, eps, op0=ALU.mult, op1=ALU.add, reads=[key], writes=[key])
    P.act(t, t, AF.Sqrt, reads=[key], writes=[key])
    P.i("dve", "reciprocal", t, t, reads=[key], writes=[key])


def build_lt():
    P = Prog()
    NTL = TPC // 128
    hT_d = P.inp("hT", [D, TPC])
    names = ["ret_f", "ret_b", "ssd_f", "ssd_b", "hy_y", "hy_vv", "hy_x0", "gdn_f", "gdn_b", "g_ret", "z_ssd", "z_gdn"]
    din = {n: P.inp(n, [128, NTL, 256]) for n in names}
    rows = {}
    for n in ("gn_w", "ssm_nw", "hy_nw", "hy_bias", "gdn_nw"):
        d = P.inp(n, [128, 256])
        t = P.sb([128, 256], F32, name=n + "_sb")
        P.dma(t[:, :], d[:, :], writes=[n])
        rows[n] = t
    wout_d = P.inp("w_out", [D, D])
    nw_d = P.inp("nw", [128, 8])
    rw_d = P.inp("rw", [128, 8, 16])
    id_d = P.inp("ident", [128, 128])
    h1_d = P.outp("h1T", [D, TPC])
    aff_d = P.outp("aff", [128, NTL, 16])
    ident = P.sb([128, 128], F32, name="ident")
    P.dma(ident[:, :], id_d[:, :], writes=["ident"])
    ones = P.sb([128, 128], F32, name="ones")
    P.i("dve", "memset", ones[:, :], 1.0, writes=["ones"])
    nw = P.sb([128, 8], F32, name="nw_sb")
    P.dma(nw[:, :], nw_d[:, :], writes=["wcol_n2"])
    rw = P.sb([128, 8, 16], F32, name="rw_sb")
    P.dma(rw[:, :, :], rw_d[:, :, :], writes=["rw"])
    wbf = [P.sb([128, D], BF16, name="wobf%d" % k) for k in range(8)]
    wst = [P.sb([128, D], F32, name="wost%d" % i) for i in range(2)]
    for k in range(8):
        P.dma(wst[k % 2][:, :], wout_d[k * 128:(k + 1) * 128, :], writes=["wost%d" % (k % 2)], eng="sync")
        P.i("pool", "tensor_copy", wbf[k][:, :], wst[k % 2][:, :], reads=["wost%d" % (k % 2)], writes=["wobf%d" % k])
    psA = PsPool(P, 2, tag="psA")
    psB = PsPool(P, 3, tag="psB")
    psT = PsPool(P, 3, tag="psT")
    NT = 1024
    hT = [P.sb([128, NT], F32, name="hT%d" % k) for k in range(8)]
    xn = [P.sb([128, NT], F32, name="xn%d" % k) for k in range(8)]
    mT = [P.sb([128, NT], BF16, name="mT%d" % k) for k in range(8)]
    inb = {n: Rot(P, 4, [128, 256], F32, "i_" + n) for n in names}
    mixed = Rot(P, 4, [128, 1024], F32, "mixed")
    tmp = Rot(P, 8, [128, 256], F32, "tmp")
    sg = Rot(P, 8, [128, 256], F32, "sg")
    st4 = Rot(P, 16, [128, 4], F32, "st4")
    st1 = Rot(P, 16, [128, 1], F32, "st1")
    affs = P.sb([128, NTL, 16], F32, name="affs")
    for t0 in range(0, TPC, NT):
        for k in range(8):
            P.dma(hT[k][:, :], hT_d[k * 128:(k + 1) * 128, t0:t0 + NT], writes=["hT%d" % k], eng="sync")
        def tile_gen(tt, t0=t0):
                tg = t0 // 128 + tt
                L_ = {}
                for i, n in enumerate(names):
                    t, tk = inb[n]()
                    P.dma(t[:, :], din[n][:, tg, :], writes=[tk], eng="sync")
                    L_[n] = (t, tk)
                mx, mxk = mixed()
                a, ak = L_["ret_f"]
                b_, bk = L_["ret_b"]
                P.i("dve", "tensor_tensor", a[:, :], a[:, :], b_[:, :], op=ALU.add, reads=[ak, bk], writes=[ak])
                a3 = a[:, :].rearrange("p (h d) -> p h d", h=4)
                s4, s4k = st4()
                P.i("dve", "tensor_reduce", s4[:, :], a3, axis=AX.X, op=ALU.add, reads=[ak], writes=[s4k])
                P.i("dve", "tensor_scalar", s4[:, :], s4[:, :], 1.0 / 64, None, op0=ALU.mult, reads=[s4k], writes=[s4k])
                P.i("dve", "tensor_tensor", a3, a3, bc3(s4[:, :], 64), op=ALU.subtract, reads=[ak, s4k], writes=[ak])
                tp, tpk = tmp()
                P.i("pool", "tensor_tensor", tp[:, :], a[:, :], a[:, :], op=ALU.mult, reads=[ak], writes=[tpk])
                v4, v4k = st4()
                P.i("dve", "tensor_reduce", v4[:, :], tp[:, :].rearrange("p (h d) -> p h d", h=4), axis=AX.X, op=ALU.add, reads=[tpk], writes=[v4k])
                rstd_small(P, v4[:, :], v4k, 1.0 / 64)
                P.i("dve", "tensor_tensor", a3, a3, bc3(v4[:, :], 64), op=ALU.mult, reads=[ak, v4k], writes=[ak])
                g_, gk = L_["g_ret"]
                s_, sk = sg()
                P.act(s_[:, :], g_[:, :], AF.Silu, reads=[gk], writes=[sk])
                P.i("pool", "tensor_tensor", a[:, :], a[:, :], rows["gn_w"][:, :], op=ALU.mult, reads=[ak, "gn_w"], writes=[ak])
                P.i("dve", "tensor_tensor", mx[:, 0:256], a[:, :], s_[:, :], op=ALU.mult, reads=[ak, sk], writes=[mxk])
                yield
                a, ak = L_["ssd_f"]
                b_, bk = L_["ssd_b"]
                z_, zk = L_["z_ssd"]
                P.i("dve", "tensor_tensor", a[:, :], a[:, :], b_[:, :], op=ALU.add, reads=[ak, bk], writes=[ak])
                s_, sk = sg()
                P.act(s_[:, :], z_[:, :], AF.Silu, reads=[zk], writes=[sk])
                P.i("dve", "tensor_tensor", a[:, :], a[:, :], s_[:, :], op=ALU.mult, reads=[ak, sk], writes=[ak])
                tp, tpk = tmp()
                s1, s1k = st1()
                P.act(tp[:, :], a[:, :], AF.Square, reads=[ak], writes=[tpk, s1k], accum_out=s1[:, 0:1])
                rstd_small(P, s1[:, :], s1k, 1.0 / 256)
                P.i("dve", "scalar_tensor_tensor", out=mx[:, 256:512], in0=a[:, :], scalar=s1[:, 0:1], in1=rows["ssm_nw"][:, :], op0=ALU.mult, op1=ALU.mult,
                    reads=[ak, s1k, "ssm_nw"], writes=[mxk])
                yield
                a, ak = L_["hy_y"]
                v_, vk = L_["hy_vv"]
                x_, xk = L_["hy_x0"]
                P.i("pool", "tensor_tensor", v_[:, :], v_[:, :], rows["hy_bias"][:, :], op=ALU.mult, reads=[vk, "hy_bias"], writes=[vk])
                P.i("dve", "tensor_tensor", a[:, :], a[:, :], v_[:, :], op=ALU.add, reads=[ak, vk], writes=[ak])
                P.i("dve", "tensor_tensor", a[:, :], a[:, :], x_[:, :], op=ALU.mult, reads=[ak, xk], writes=[ak])
                tp, tpk = tmp()
                s1, s1k = st1()
                P.act(tp[:, :], a[:, :], AF.Square, reads=[ak], writes=[tpk, s1k], accum_out=s1[:, 0:1])
                rstd_small(P, s1[:, :], s1k, 1.0 / 256)
                P.i("dve", "scalar_tensor_tensor", out=mx[:, 512:768], in0=a[:, :], scalar=s1[:, 0:1], in1=rows["hy_nw"][:, :], op0=ALU.mult, op1=ALU.mult,
                    reads=[ak, s1k, "hy_nw"], writes=[mxk])
                yield
                a, ak = L_["gdn_f"]
                b_, bk = L_["gdn_b"]
                z_, zk = L_["z_gdn"]
                P.i("dve", "tensor_tensor", a[:, :], a[:, :], b_[:, :], op=ALU.add, reads=[ak, bk], writes=[ak])
                a3 = a[:, :].rearrange("p (h d) -> p h d", h=4)
                tp, tpk = tmp()
                P.i("pool", "tensor_tensor", tp[:, :], a[:, :], a[:, :], op=ALU.mult, reads=[ak], writes=[tpk])
                v4, v4k = st4()
                P.i("dve", "tensor_reduce", v4[:, :], tp[:, :].rearrange("p (h d) -> p h d", h=4), axis=AX.X, op=ALU.add, reads=[tpk], writes=[v4k])
                rstd_small(P, v4[:, :], v4k, 1.0 / 64)
                P.i("dve", "tensor_tensor", a3, a3, bc3(v4[:, :], 64), op=ALU.mult, reads=[ak, v4k], writes=[ak])
                s_, sk = sg()
                P.act(s_[:, :], z_[:, :], AF.Silu, reads=[zk], writes=[sk])
                P.i("pool", "tensor_tensor", a[:, :], a[:, :], rows["gdn_nw"][:, :], op=ALU.mult, reads=[ak, "gdn_nw"], writes=[ak])
                P.i("dve", "tensor_tensor", mx[:, 768:1024], a[:, :], s_[:, :], op=ALU.mult, reads=[ak, sk], writes=[mxk])
                yield
                for k in range(8):
                    pt, pk = psT()
                    P.tr(pt[:, :128], mx[:, k * 128:(k + 1) * 128], ident[:, :], reads=[mxk, "ident"], writes=[pk])
                    if k % 2 == 0:
                        P.act(mT[k][:, tt * 128:(tt + 1) * 128], pt[:, :128], AF.Copy, reads=[pk], writes=["mT%d" % k])
                    else:
                        P.i("dve", "tensor_copy", mT[k][:, tt * 128:(tt + 1) * 128], pt[:, :128], reads=[pk], writes=["mT%d" % k])

        NTI = 3
        tts = list(range(NT // 128))
        ti, act_ = 0, []
        while ti < len(tts) or act_:
            while len(act_) < NTI and ti < len(tts):
                act_.append(tile_gen(tts[ti]))
                ti += 1
            for gq in list(act_):
                if next(gq, "DONE") == "DONE":
                    act_.remove(gq)
        for m in range(8):
            for n0 in range(0, NT, 512):
                p, pk = psB()
                for k in range(8):
                    P.mm(p[:, :], wbf[k][:, m * 128:(m + 1) * 128], mT[k][:, n0:n0 + 512], start=(k == 0), stop=(k == 7),
                         reads=["wobf%d" % k, "mT%d" % k], writes=[pk])
                P.i("dve", "tensor_tensor", hT[m][:, n0:n0 + 512], hT[m][:, n0:n0 + 512], p[:, :], op=ALU.add, reads=["hT%d" % m, pk], writes=["hT%d" % m])
            P.dma(h1_d[m * 128:(m + 1) * 128, t0:t0 + NT], hT[m][:, :], reads=["hT%d" % m], writes=["h1_d"], eng="pool")
        rmsnorm_fm(P, [h[:, :] for h in hT], nw, [x[:, :] for x in xn], NT, ones, psA, "n2",
                   ["xn%d" % k for k in range(8)], ["hT%d" % k for k in range(8)])
        for tt in range(NT // 128):
            tg = t0 // 128 + tt
            p, pk = psT()
            for k in range(8):
                P.mm(p[:, :16], xn[k][:, tt * 128:(tt + 1) * 128], rw[:, k, :], start=(k == 0), stop=(k == 7), reads=["xn%d" % k, "rw"], writes=[pk])
            m1, m1k = st1()
            P.i("dve", "tensor_reduce", m1[:, :], p[:, :16], axis=AX.X, op=ALU.max, negate=True, reads=[pk], writes=[m1k])
            s1, s1k = st1()
            P.act(affs[:, tg, :], p[:, :16], AF.Exp, reads=[pk, m1k], writes=["affs", s1k], bias=m1[:, 0:1], accum_out=s1[:, 0:1])
            P.i("dve", "reciprocal", s1[:, :], s1[:, :], reads=[s1k], writes=[s1k])
            P.i("dve", "tensor_scalar", affs[:, tg, :], affs[:, tg, :], s1[:, 0:1], None, op0=ALU.mult, reads=["affs", s1k], writes=["affs"])
    P.dma(aff_d[:, :, :], affs[:, :, :], reads=["affs"], writes=["aff_d"])
    return P


def tm_tiles(a, ntile):
    return np.ascontiguousarray(a.reshape(ntile, 128, -1).transpose(1, 0, 2))


def un_tm(a):
    return a.transpose(1, 0, 2).reshape(a.shape[0] * a.shape[1], -1)


def bcast_rows(v, n=128):
    return np.ascontiguousarray(np.broadcast_to(v.reshape(1, -1), (n, v.size)))


def lt_inmaps(hT, arr, gn_w, ssm_nw, hy_nw, hy_bias, gdn_nw, w_out, nfw, rw):
    maps = []
    ident = np.eye(128, dtype=np.float32)
    NTL = TPC // 128
    for c in range(NCORES):
        sl = slice(c * TPC, (c + 1) * TPC)
        m = {"hT": np.ascontiguousarray(hT[:, sl]), "w_out": w_out, "ident": ident,
             "nw": np.ascontiguousarray(nfw.reshape(8, 128).T), "rw": np.ascontiguousarray(rw.reshape(8, 128, 16).transpose(1, 0, 2)),
             "gn_w": bcast_rows(gn_w), "ssm_nw": bcast_rows(ssm_nw), "hy_nw": bcast_rows(hy_nw), "hy_bias": bcast_rows(hy_bias),
             "gdn_nw": bcast_rows(np.tile(gdn_nw, 4))}
        for n, a in arr.items():
            m[n] = tm_tiles(a[sl], NTL)
        maps.append(m)
    return maps


def lt_collect(res):
    h1T = np.concatenate([r["h1T"] for r in res], axis=1)
    aff = np.concatenate([un_tm(r["aff"]) for r in res], axis=0)
    return h1T, aff


CAP = 2 * L // 16
NBIS = 22


def build_le(final):
    P = Prog()
    h1_d = P.inp("h1T", [D, TPC])
    affall_d = P.inp("aff_all", [128, 16, L // 128])
    affT_d = P.inp("affT", [16, TPC])
    id_d = P.inp("ident", [128, 128])
    nw_d = P.inp("nw", [128, 8])
    pnw_d = P.inp("pnw", [128, 8])
    wg_d = P.inp("wg", [16, D, D])
    wu_d = P.inp("wu", [16, D, D])
    wd_d = P.inp("wd", [16, D, D])
    pg_d = P.inp("pgw", [D, D])
    pp_d = P.inp("ppw", [256, D])
    pT_d = P.inp("pT", [256, TPC])
    out_d = P.outp("h3T", [D, TPC])
    if final:
        fnw_d = P.inp("fnw", [128, 8])
    ident = P.sb([128, 128], F32, name="ident")
    P.dma(ident[:, :], id_d[:, :], writes=["ident"])
    ones = P.sb([128, 128], F32, name="ones")
    P.i("dve", "memset", ones[:, :], 1.0, writes=["ones"])
    nw = P.sb([128, 8], F32, name="nw_sb")
    P.dma(nw[:, :], nw_d[:, :], writes=["wcol_n2"])
    pnw = P.sb([128, 8], F32, name="pnw_sb")
    P.dma(pnw[:, :], pnw_d[:, :], writes=["wcol_n3"])
    if final:
        fnw = P.sb([128, 8], F32, name="fnw_sb")
        P.dma(fnw[:, :], fnw_d[:, :], writes=["wcol_n4"])
    selt = Rot(P, 2, [16, 128], F32, "selt")
    psA = PsPool(P, 2, tag="psA")
    psB = PsPool(P, 4, tag="psB")
    psC = PsPool(P, 2, tag="psC")
    NB = L // 128
    big = P.sb([128, 2, 16 * NB], F32, name="big")
    aall = big[:, 0, :].rearrange("p (e n) -> p e n", e=16)
    cmp_ = big[:, 1, :].rearrange("p (e n) -> p e n", e=16)
    P.dma(aall, affall_d[:, :, :], writes=["aall"])
    lo = P.sb([128, 16], F32, name="lo")
    hi = P.sb([128, 16], F32, name="hi")
    mid = P.sb([128, 16], F32, name="mid")
    cnt = P.sb([128, 16], F32, name="cnt")
    ge = P.sb([128, 16], F32, name="ge")
    d1 = P.sb([128, 16], F32, name="d1")
    P.i("dve", "memset", lo[:, :], 0.0, writes=["lo"])
    P.i("dve", "memset", hi[:, :], 2.0, writes=["hi"])
    P.i("dve", "memset", mid[:, :], 0.5, writes=["mid"])
    for it in range(NBIS):
        P.i("dve", "tensor_tensor", cmp_, aall, bc3(mid[:, :], NB), op=ALU.is_ge, reads=["aall", "mid"], writes=["cmp"])
        P.i("dve", "tensor_reduce", cnt[:, :], cmp_, axis=AX.X, op=ALU.add, reads=["cmp"], writes=["cnt"])
        p, pk = psC()
        P.mm(p[:, :16], ones[:, :], cnt[:, :], reads=["ones", "cnt"], writes=[pk])
        P.i("dve", "tensor_scalar", ge[:, :], p[:, :16], CAP - 0.5, None, op0=ALU.is_ge, reads=[pk], writes=["ge"])
        P.i("dve", "tensor_tensor", d1[:, :], mid[:, :], lo[:, :], op=ALU.subtract, reads=["mid", "lo"], writes=["d1"])
        P.i("dve", "tensor_tensor", d1[:, :], d1[:, :], ge[:, :], op=ALU.mult, reads=["d1", "ge"], writes=["d1"])
        P.i("dve", "tensor_tensor", lo[:, :], lo[:, :], d1[:, :], op=ALU.add, reads=["lo", "d1"], writes=["lo"])
        P.i("dve", "tensor_tensor", d1[:, :], hi[:, :], mid[:, :], op=ALU.subtract, reads=["mid", "hi"], writes=["d1"])
        P.i("dve", "tensor_tensor", d1[:, :], d1[:, :], ge[:, :], op=ALU.mult, reads=["d1", "ge"], writes=["d1"])
        P.i("dve", "tensor_tensor", hi[:, :], mid[:, :], d1[:, :], op=ALU.add, reads=["mid", "d1"], writes=["hi"])
        P.i("dve", "tensor_tensor", mid[:, :], lo[:, :], hi[:, :], op=ALU.add, reads=["lo", "hi"], writes=["mid"])
        P.i("dve", "tensor_scalar", mid[:, :], mid[:, :], 0.5, None, op0=ALU.mult, reads=["mid"], writes=["mid"])
    thc = P.sb([16, 1], F32, name="thc")
    P.i("dve", "tensor_tensor", d1[:16, :], lo[:16, :], ident[:16, :16], op=ALU.mult, reads=["lo", "ident"], writes=["d1"])
    P.i("dve", "tensor_reduce", thc[:, :], d1[:16, :], axis=AX.X, op=ALU.add, reads=["d1"], writes=["thc"])
    gwT = P.sb([16, TPC], F32, name="gwT")
    P.dma(gwT[:, :], affT_d[:, :], writes=["gwT"])
    P.i("dve", "scalar_tensor_tensor", out=gwT[:, :], in0=gwT[:, :], scalar=thc[:, 0:1], in1=gwT[:, :], op0=ALU.is_ge, op1=ALU.mult, reads=["gwT", "thc"], writes=["gwT"])
    NT = 1024
    hT = [P.sb([128, NT], F32, name="hT%d" % k) for k in range(8)]
    xn = [P.sb([128, NT], BF16, name="xn%d" % k) for k in range(8)]
    wbuf = Rot(P, 4, [128, 8, D], BF16, "wbuf")
    wstg = Rot(P, 6, [128, D], F32, "wstg")
    actb = Rot(P, 2, [128, 8, 512], BF16, "actb")
    gwb = Rot(P, 2, [128, 512], F32, "gwb")
    sgl = Rot(P, 2, [128, 512], F32, "sgl")
    u2 = Rot(P, 2, [128, 512], F32, "u2")
    cvi = [0]

    def load_w(src, e):
        wb, wbk = wbuf()
        for k in range(8):
            st, stk = wstg()
            P.dma(st[:, :], src[e, k * 128:(k + 1) * 128, :], writes=[stk], eng="sync")
            ce = ("dve", "act", "pool", "dve", "act")[cvi[0] % 5]
            cvi[0] += 1
            if ce == "act":
                P.act(wb[:, k, :], st[:, :], AF.Copy, reads=[stk], writes=[wbk])
            else:
                P.i(ce, "tensor_copy", wb[:, k, :], st[:, :], reads=[stk], writes=[wbk])
        return wb, wbk

    for t0 in range(0, TPC, NT):
        for k in range(8):
            P.dma(hT[k][:, :], h1_d[k * 128:(k + 1) * 128, t0:t0 + NT], writes=["hT%d" % k], eng=("sync", "act")[k % 2])
        rmsnorm_fm(P, [h[:, :] for h in hT], nw, [x[:, :] for x in xn], NT, ones, psA, "n2",
                   ["xn%d" % k for k in range(8)], ["hT%d" % k for k in range(8)])
        for e in range(16):
            wg, wgk = load_w(wg_d, e)
            wu, wuk = load_w(wu_d, e)
            wd, wdk = load_w(wd_d, e)
            se, sek = selt()
            P.i("dve", "tensor_scalar", se[:, :], ones[:16, :], ident[:16, e:e + 1], None, op0=ALU.mult, reads=["ones", "ident"], writes=[sek])
            for n0 in range(0, NT, 512):
                pb, pbk = psC()
                P.mm(pb[:, :], se[:, :], gwT[:, t0 + n0:t0 + n0 + 512], reads=[sek, "gwT"], writes=[pbk])
                gb, gbk = gwb()
                P.act(gb[:, :], pb[:, :], AF.Copy, reads=[pbk], writes=[gbk])
                ab, abk = actb()
                for f in range(8):
                    pg, pgk = psB()
                    for k in range(8):
                        P.mm(pg[:, :], wg[:, k, f * 128:(f + 1) * 128], xn[k][:, n0:n0 + 512], start=(k == 0), stop=(k == 7), reads=[wgk, "xn%d" % k], writes=[pgk])
                    pu, puk = psB()
                    for k in range(8):
                        P.mm(pu[:, :], wu[:, k, f * 128:(f + 1) * 128], xn[k][:, n0:n0 + 512], start=(k == 0), stop=(k == 7), reads=[wuk, "xn%d" % k], writes=[puk])
                    s_, sk = sgl()
                    P.act(s_[:, :], pg[:, :], AF.Silu, reads=[pgk], writes=[sk])
                    u_, uk = u2()
                    P.i("dve", "tensor_tensor", u_[:, :], pu[:, :], gb[:, :], op=ALU.mult, reads=[puk, gbk], writes=[uk])
                    P.i("dve", "tensor_tensor", ab[:, f, :], s_[:, :], u_[:, :], op=ALU.mult, reads=[sk, uk], writes=[abk])
                for m in range(8):
                    pd, pdk = psB()
                    for f in range(8):
                        P.mm(pd[:, :], wd[:, f, m * 128:(m + 1) * 128], ab[:, f, :], start=(f == 0), stop=(f == 7), reads=[wdk, abk], writes=[pdk])
                    P.i("dve", "tensor_tensor", hT[m][:, n0:n0 + 512], hT[m][:, n0:n0 + 512], pd[:, :], op=ALU.add, reads=[pdk, "hT%d" % m], writes=["hT%d" % m])
        rmsnorm_fm(P, [h[:, :] for h in hT], pnw, [x[:, :] for x in xn], NT, ones, psA, "n3",
                   ["xn%d" % k for k in range(8)], ["hT%d" % k for k in range(8)])
        pgw, pgwk = load_w(pg_d.rearrange("(o a) b -> o a b", o=1), 0)
        if t0 == 0:
            ppw = P.sb([128, 2, D], BF16, name="ppw")
            for k in range(2):
                st, stk = wstg()
                P.dma(st[:, :], pp_d[k * 128:(k + 1) * 128, :], writes=[stk])
                P.i("pool", "tensor_copy", ppw[:, k, :], st[:, :], reads=[stk], writes=["ppw"])
            pTb = P.sb([128, 2, NT], BF16, name="pTb")
        pst = big[:, 0, 0:NT]
        for k in range(2):
            P.dma(pst, pT_d[k * 128:(k + 1) * 128, t0:t0 + NT], reads=["cmp"], writes=["aall"])
            P.act(pTb[:, k, :], pst, AF.Copy, reads=["aall"], writes=["pTb"])
        for m in range(8):
            for n0 in range(0, NT, 512):
                pg, pgk = psB()
                for k in range(8):
                    P.mm(pg[:, :], pgw[:, k, m * 128:(m + 1) * 128], xn[k][:, n0:n0 + 512], start=(k == 0), stop=(k == 7), reads=[pgwk, "xn%d" % k], writes=[pgk])
                pp, ppk = psB()
                for k in range(2):
                    P.mm(pp[:, :], ppw[:, k, m * 128:(m + 1) * 128], pTb[:, k, n0:n0 + 512], start=(k == 0), stop=(k == 1), reads=["ppw", "pTb"], writes=[ppk])
                s_, sk = sgl()
                P.act(s_[:, :], pg[:, :], AF.Sigmoid, reads=[pgk], writes=[sk])
                u_, uk = u2()
                P.i("dve", "tensor_tensor", u_[:, :], pp[:, :], s_[:, :], op=ALU.mult, reads=[ppk, sk], writes=[uk])
                P.i("pool", "tensor_tensor", hT[m][:, n0:n0 + 512], hT[m][:, n0:n0 + 512], u_[:, :], op=ALU.add, reads=["hT%d" % m, uk], writes=["hT%d" % m])
        if final:
            rmsnorm_fm(P, [h[:, :] for h in hT], fnw, [h[:, :] for h in hT], NT, ones, psA, "n4",
                       ["hT%d" % k for k in range(8)], ["hT%d" % k for k in range(8)])
            for m in range(8):
                P.dma(out_d[m * 128:(m + 1) * 128, t0:t0 + NT], hT[m][:, :], reads=["hT%d" % m], writes=["out_d"], eng=("sync", "act")[m % 2])
        else:
            for m in range(8):
                P.dma(out_d[m * 128:(m + 1) * 128, t0:t0 + NT], hT[m][:, :], reads=["hT%d" % m], writes=["out_d"], eng=("sync", "act")[m % 2])
    return P


def le_inmaps(h1T, aff, nfw, pnw, wg, wu, wd, pgw, ppw, pT, fnw=None):
    ident = np.eye(128, dtype=np.float32)
    aff_all = np.ascontiguousarray(aff.reshape(128, L // 128, 16).transpose(0, 2, 1))
    affT = aff.T
    colw = lambda w: np.ascontiguousarray(w.reshape(8, 128).T)
    maps = []
    for c in range(NCORES):
        sl = slice(c * TPC, (c + 1) * TPC)
        m = {"h1T": np.ascontiguousarray(h1T[:, sl]), "aff_all": aff_all, "affT": np.ascontiguousarray(affT[:, sl]), "ident": ident,
             "nw": colw(nfw), "pnw": colw(pnw), "wg": wg, "wu": wu, "wd": wd, "pgw": pgw, "ppw": ppw, "pT": np.ascontiguousarray(pT[:, sl])}
        if fnw is not None:
            m["fnw"] = colw(fnw)
        maps.append(m)
    return maps


def _colw(w):
    return np.ascontiguousarray(w.reshape(8, 128).T)


def _scan_consts():
    tri = np.triu(np.ones((128, 128), np.float32))
    ident = np.eye(128, dtype=np.float32)
    mneg_incl = np.where(tri > 0, 0.0, NEG).astype(np.float32)
    mneg_strict = np.where(np.triu(np.ones((128, 128), np.float32), 1) > 0, 0.0, NEG).astype(np.float32)
    pmT = np.where(np.tril(np.ones((128, 128), np.float32), -1) > 0, 0.0, -NEG).astype(np.float32)
    return tri, ident, mneg_incl, mneg_strict, pmT


def _rope_tables():
    inv = (10000.0 ** (-np.arange(0, 64, 2, dtype=np.float32) / 64)).astype(np.float32)
    ang = (np.arange(L, dtype=np.float32)[:, None] * inv[None]).astype(np.float32)
    cos, sin = np.cos(ang), np.sin(ang)
    cos2 = np.concatenate([cos, cos], 1).astype(np.float32)
    sinS = np.concatenate([-sin, sin], 1).astype(np.float32)
    return cos2, sinS


def _fm_pad(rows_fm, flip, pad):
    a = rows_fm[:, ::-1] if flip else rows_fm
    return np.ascontiguousarray(np.pad(a, ((0, 0), (pad, pad))))


def _col_tm(v, flip):
    if flip:
        v = v[::-1]
    return np.ascontiguousarray(v.reshape(L // 128, 128).T)


def _full(v):
    return np.full((128, 1), v, np.float32)


def run_ret(colsT):
    tri, ident, mi, ms, _ = _scan_consts()
    cos2, sinS = _rope_tables()
    nch = L // 128
    lg = np.log(1.0 - 2.0 ** (-5.0 - np.arange(4, dtype=np.float32))).astype(np.float32)
    maps = []
    for c in range(NCORES):
        h, dr = c // 2, c % 2

        def prep(a_tm):
            return tm_tiles(a_tm[::-1] if dr else a_tm, nch)

        qh = colsT[64 * h:64 * h + 64].T
        kh = colsT[256 + 64 * h:256 + 64 * h + 64].T
        vh = colsT[512 + 64 * h:512 + 64 * h + 64].T
        sw = lambda a: np.concatenate([a[:, 32:], a[:, :32]], 1)
        maps.append({"tri": tri, "ident": ident, "mneg": mi if dr == 0 else ms,
                     "q": prep(qh), "qsw": prep(sw(qh)), "k": prep(kh), "ksw": prep(sw(kh)), "v": prep(vh),
                     "cq": prep(cos2), "sq": prep(sinS), "ck": prep(cos2 * np.float32(0.125)), "sk": prep(sinS * np.float32(0.125)),
                     "g": np.full((128, nch), lg[h], np.float32)})
    res = run_spmd(build_scan("ret", L), maps)
    return _collect_scan(res)


def _collect_scan(res):
    f = np.zeros((L, 256), np.float32)
    b = np.zeros((L, 256), np.float32)
    for c in range(NCORES):
        h, dr = c // 2, c % 2
        o = un_tm(res[c]["o"])
        if dr:
            b[:, 64 * h:64 * h + 64] = o[::-1]
        else:
            f[:, 64 * h:64 * h + 64] = o
    return f, b


def run_ssd(cs, conv_w, conv_b, a_log, dt_bias, d_skip):
    tri, ident, mi, _, _ = _scan_consts()
    maps = []
    for c in range(NCORES):
        h, dr = c // 2, c % 2
        gi = h // 2
        xr = np.arange(256 + 64 * h, 256 + 64 * h + 64)
        br = np.arange(512 + 128 * gi, 512 + 128 * gi + 128)
        cr = np.arange(768 + 128 * gi, 768 + 128 * gi + 128)

        def cw(rows):
            w = conv_w[:, rows - 256].T
            return np.ascontiguousarray(w[:, ::-1] if dr else w)

        cb = lambda rows: np.ascontiguousarray(conv_b[rows - 256][:, None])
        maps.append({"tri": tri, "ident": ident, "mneg": mi,
                     "xpad": _fm_pad(cs[xr], dr, 2), "bpad": _fm_pad(cs[br], dr, 2), "cpad": _fm_pad(cs[cr], dr, 2),
                     "cwx": cw(xr), "cbx": cb(xr), "cwb": cw(br), "cbb": cb(br), "cwc": cw(cr), "cbc": cb(cr),
                     "dtb": _full(dt_bias[dr, h]), "alog": _full(a_log[dr, h]),
                     "dsk": _full(d_skip[h]) if dr == 0 else np.zeros((128, 1), np.float32),
                     "dtraw": _col_tm(cs[1024 + 4 * dr + h], dr)})
    res = run_spmd(build_scan("ssd", L), maps)
    return _collect_scan(res)


def run_gdn(cg, conv_w, a_log, dt_bias):
    tri, ident, mi, _, pmT = _scan_consts()
    maps = []
    for c in range(NCORES):
        h, dr = c // 2, c % 2
        qr = np.arange(64 * h, 64 * h + 64)

        def cw(rows):
            w = conv_w[:, rows].T
            return np.ascontiguousarray(w[:, ::-1] if dr else w)

        maps.append({"tri": tri, "ident": ident, "mneg": mi, "pmT": pmT,
                     "qpad": _fm_pad(cg[qr], dr, 2), "kpad": _fm_pad(cg[256 + qr], dr, 2), "vpad": _fm_pad(cg[512 + qr], dr, 2),
                     "cwq": cw(qr), "cwk": cw(256 + qr), "cwv": cw(512 + qr),
                     "dtb": _full(dt_bias[dr, h]), "alog": _full(a_log[dr, h]),
                     "araw": _col_tm(cg[1024 + 4 * dr + h], dr), "braw": _col_tm(cg[1032 + 4 * dr + h], dr)})
    res = run_spmd(build_scan("gdn", L), maps)
    return _collect_scan(res)


def kernel(x, p, norm_mix_w, w_in, ret_gn_w, ssm_conv_w, ssm_conv_b, ssm_a_log, ssm_dt_bias,
           ssm_d, ssm_norm_w, hy_conv_w, hy_conv_b, hy_filt_w_in, hy_filt_b_in, hy_filt_w_mid,
           hy_filt_b_mid, hy_filt_freq, hy_filt_w_out, hy_bias, hy_norm_w, gdn_conv_w, gdn_a_log,
           gdn_dt_bias, gdn_norm_w, w_out, norm_ffn_w, router_w, exp_w_gate, exp_w_up, exp_w_down,
           ple_norm_w, ple_gate_w, ple_proj_w, final_norm_w):
    A = lambda a: np.asarray(a, dtype=np.float32)
    hT = np.ascontiguousarray(A(x)[0].T)
    depth = A(w_in).shape[0]
    for i in range(depth):
        maps = [{"hT": np.ascontiguousarray(hT[:, c * TPC:(c + 1) * TPC]), "nw": _colw(A(norm_mix_w)[i]), "w_in": np.ascontiguousarray(A(w_in)[i])}
                for c in range(NCORES)]
        res = run_spmd(build_inproj(), maps)
        colsT = np.concatenate([r["colsT"] for r in res], axis=1)
        c_ret, c_ssm, c_hy, c_gdn = colsT[0:1024], colsT[1024:2056], colsT[2056:2824], colsT[2824:3864]
        ret_f, ret_b = run_ret(c_ret)
        ssd_f, ssd_b = run_ssd(c_ssm, A(ssm_conv_w)[i], A(ssm_conv_b)[i], A(ssm_a_log)[i], A(ssm_dt_bias)[i], A(ssm_d)[i])
        gdn_f, gdn_b = run_gdn(c_gdn, A(gdn_conv_w)[i], A(gdn_a_log)[i], A(gdn_dt_bias)[i])
        hmaps = hyena_inmaps(c_hy, A(hy_conv_w)[i], A(hy_conv_b)[i], A(hy_filt_w_in)[i], A(hy_filt_b_in)[i], A(hy_filt_w_mid)[i],
                             A(hy_filt_b_mid)[i], A(hy_filt_freq)[i], A(hy_filt_w_out)[i], L)
        hy_y, hy_vv, hy_x0 = hyena_collect(run_spmd(build_hyena(L), hmaps), L)
        arr = {"ret_f": ret_f, "ret_b": ret_b, "ssd_f": ssd_f, "ssd_b": ssd_b, "hy_y": hy_y, "hy_vv": hy_vv, "hy_x0": hy_x0,
               "gdn_f": gdn_f, "gdn_b": gdn_b, "g_ret": c_ret[768:1024].T, "z_ssd": c_ssm[0:256].T, "z_gdn": c_gdn[768:1024].T}
        maps = lt_inmaps(hT, arr, A(ret_gn_w)[i], A(ssm_norm_w)[i], A(hy_norm_w)[i], A(hy_bias)[i], A(gdn_norm_w)[i],
                         np.ascontiguousarray(A(w_out)[i]), A(norm_ffn_w)[i], A(router_w)[i])
        h1T, aff = lt_collect(run_spmd(build_lt(), maps))
        final = i == depth - 1
        maps = le_inmaps(h1T, aff, A(norm_ffn_w)[i], A(ple_norm_w)[i], np.ascontiguousarray(A(exp_w_gate)[i]), np.ascontiguousarray(A(exp_w_up)[i]),
                         np.ascontiguousarray(A(exp_w_down)[i]), np.ascontiguousarray(A(ple_gate_w)[i]), np.ascontiguousarray(A(ple_proj_w)[i]),
                         np.ascontiguousarray(A(p)[i, 0].T), A(final_norm_w) if final else None)
        res = run_spmd(build_le(final), maps)
        hT = np.concatenate([r["h3T"] for r in res], axis=1)
    return np.ascontiguousarray(hT.T)[None].astype(np.float32)
```
